# Optimizing a Trainium2 kernel written in Bass

```python
import math
import jax, jax.numpy as jnp
from jax import lax
import numpy as np

D_MODEL = 4096
BATCH = 1
SEQ = 8192
DEPTH = 2

N_A_LAYERS = DEPTH // 2
N_B_LAYERS = DEPTH - N_A_LAYERS
EPS = 1e-6
NEG = -1e30
FORCE = 1e30
A_WIDTH = D_MODEL
A_CHUNK = 128
A_GROUPS = 32
A_GROUP_DIM = A_WIDTH // A_GROUPS
HEAD_DIM = 128
N_HEADS = D_MODEL // HEAD_DIM
N_KV_GROUPS = 4
HEADS_PER_GROUP = N_HEADS // N_KV_GROUPS
CMP_LEN = 32
CMP_STRIDE = 16
SLC_BLOCK = 64
SLC_TOPN = 16
WINDOW = 512
Q_BLOCK = 128
N_BRANCH = 3
N_BUCKETS = 32
MAX_DISTANCE = 1024
D_FF = 11008
CONV_WIDTH = 3

kernel_name = "yoco_gmlp_nsa_convffn_hybrid"


def rmsnorm(x, g):
    xf = x.astype(jnp.float32)
    y = xf * lax.rsqrt(jnp.mean(xf * xf, axis=-1, keepdims=True) + EPS)
    return (y * g.astype(jnp.float32)).astype(x.dtype)


def layernorm(x, g, b):
    xf = x.astype(jnp.float32)
    mu = jnp.mean(xf, axis=-1, keepdims=True)
    var = jnp.mean(jnp.square(xf - mu), axis=-1, keepdims=True)
    y = (xf - mu) * lax.rsqrt(var + EPS)
    return (y * g.astype(jnp.float32) + b.astype(jnp.float32)).astype(x.dtype)


def t5_bucket(rel):
    n = jnp.maximum(rel, 0)
    max_exact = N_BUCKETS // 2
    nf = jnp.maximum(n, max_exact).astype(jnp.float32)
    large = max_exact + (jnp.log(nf / max_exact) / math.log(MAX_DISTANCE / max_exact)
                         * (N_BUCKETS - max_exact)).astype(jnp.int32)
    large = jnp.minimum(large, N_BUCKETS - 1)
    return jnp.where(n < max_exact, n, large)


def residual_sublayer(x, c_act, w_mod, b_mod, g_pre, g_post, fn):
    shift, scale, gate = jnp.split(c_act @ w_mod + b_mod, 3, axis=-1)
    h = rmsnorm(x, g_pre) * (1 + scale[:, None, :]) + shift[:, None, :]
    return x + gate[:, None, :] * rmsnorm(fn(h), g_post)


def chunked_gmlp(h, w_in, b_in, ln_g, ln_b, w_s, b_s, w_out):
    B, S, _ = h.shape
    z = jax.nn.gelu(h @ w_in + b_in)
    u, v = jnp.split(z, 2, axis=-1)
    v = layernorm(v, ln_g, ln_b)
    v = v.reshape(B, S // A_CHUNK, A_CHUNK, A_GROUPS, A_GROUP_DIM)
    causal = jnp.tril(jnp.ones((A_CHUNK, A_CHUNK), dtype=bool))
    ws = jnp.where(causal[None], w_s, 0)
    mixed = jnp.einsum('gts,bcsgd->bctgd', ws, v) + b_s.T[None, None, :, :, None]
    return (u * mixed.reshape(B, S, A_WIDTH)) @ w_out


def conv_ffn(h, w_gate, w_up, conv_w, conv_b, w_down):
    S = h.shape[1]
    a = h @ w_gate
    ap = jnp.pad(a, ((0, 0), (CONV_WIDTH - 1, 0), (0, 0)))
    a = sum(ap[:, k:k + S] * conv_w[k] for k in range(CONV_WIDTH)) + conv_b
    return (jax.nn.silu(a) * (h @ w_up)) @ w_down


def compress_blocks(raw, pe, w1, b1, w2):
    B, S, G, DH = raw.shape
    m_len = CMP_LEN // CMP_STRIDE
    n_w = S // CMP_STRIDE
    n_full = n_w - m_len + 1
    r = raw.reshape(B, n_w, CMP_STRIDE, G, DH)
    blocks = jnp.concatenate([r[:, i:i + n_full] for i in range(m_len)], axis=2)
    blocks = blocks + pe[None, None, :, None, :]
    flat = blocks.transpose(0, 1, 3, 2, 4).reshape(B, n_full, G, CMP_LEN * DH)
    out = jax.nn.gelu(flat @ w1 + b1) @ w2
    return jnp.pad(out, ((0, 0), (0, m_len - 1), (0, 0), (0, 0)))


def shared_kv(xs, c_act, kv_norm, kv_mod_w, kv_mod_b, w_kv, cmp_pe, cmp_w1, cmp_b1, cmp_w2):
    B, S, _ = xs.shape
    G, DH = N_KV_GROUPS, HEAD_DIM
    shift, scale = jnp.split(c_act @ kv_mod_w + kv_mod_b, 2, axis=-1)
    h = rmsnorm(xs, kv_norm) * (1 + scale[:, None, :]) + shift[:, None, :]
    kv = (h @ w_kv).reshape(B, S, 2 * N_BRANCH, G, DH)
    kc = compress_blocks(kv[:, :, 0], cmp_pe[0], cmp_w1[0], cmp_b1[0], cmp_w2[0])
    vc = compress_blocks(kv[:, :, 1], cmp_pe[1], cmp_w1[1], cmp_b1[1], cmp_w2[1])
    n_slc = S // SLC_BLOCK
    to_blocks = lambda t: t.reshape(B, n_slc, SLC_BLOCK, G, DH).transpose(0, 3, 1, 2, 4)
    k_slc, v_slc = to_blocks(kv[:, :, 2]), to_blocks(kv[:, :, 3])
    pad_w = lambda t: jnp.pad(t, ((0, 0), (WINDOW, 0), (0, 0), (0, 0)))
    k_win, v_win = pad_w(kv[:, :, 4]), pad_w(kv[:, :, 5])
    return (kc, vc, k_slc, v_slc, k_win, v_win)


_gather_blocks = jax.vmap(jax.vmap(lambda kb, ix: kb[ix]))


def nsa_attention(h, kvs, w_in, b_in, w_out, rel_bias):
    B, S, _ = h.shape
    G, HG, DH = N_KV_GROUPS, HEADS_PER_GROUP, HEAD_DIM
    kc, vc, k_slc, v_slc, k_win, v_win = kvs
    proj = h @ w_in + b_in
    q = proj[..., :N_HEADS * DH].reshape(B, S, G, HG, DH)
    gates = jax.nn.sigmoid(proj[..., N_HEADS * DH:].astype(jnp.float32)).reshape(B, S, G, HG, N_BRANCH)
    nq = S // Q_BLOCK
    q_blocks = jnp.moveaxis(q.reshape(B, nq, Q_BLOCK, G, HG, DH), 1, 0)
    g_blocks = jnp.moveaxis(gates.reshape(B, nq, Q_BLOCK, G, HG, N_BRANCH), 1, 0)

    n_cmp = kc.shape[1]
    n_slc = S // SLC_BLOCK
    n_sel = min(SLC_TOPN, n_slc)
    m_sub = SLC_BLOCK // CMP_STRIDE
    m_len = CMP_LEN // CMP_STRIDE
    cmp_end = jnp.arange(n_cmp) * CMP_STRIDE + CMP_LEN - 1
    tb = rel_bias.reshape(N_BUCKETS, G, HG)
    g_ix = jnp.arange(G)[None, :, None, None]
    scale = HEAD_DIM ** -0.5
    t_loc = jnp.arange(Q_BLOCK)
    k_loc = jnp.arange(Q_BLOCK + WINDOW)
    win_rel = t_loc[:, None] + WINDOW - k_loc[None, :]
    win_bias = tb[t5_bucket(win_rel)].transpose(2, 3, 0, 1)
    blk = jnp.arange(n_slc)

    def block_fn(args):
        qb, gb, bi = args
        q0 = bi * Q_BLOCK
        t = q0 + t_loc
        rel_c = t[:, None] - cmp_end[None, :]
        valid_c = rel_c >= 0
        s_c = jnp.einsum('btghd,bngd->bghtn', qb, kc).astype(jnp.float32) * scale
        s_c = jnp.where(valid_c, s_c + tb[t5_bucket(rel_c)].transpose(2, 3, 0, 1), NEG)
        p_c = jax.nn.softmax(s_c, axis=-1) * valid_c
        o_c = jnp.einsum('bghtn,bngd->btghd', p_c.astype(vc.dtype), vc)
        imp = p_c.sum(axis=2)
        imp_slc = imp.reshape(B, G, Q_BLOCK, n_slc, m_sub).sum(-1)
        imp_pad = jnp.pad(imp, ((0, 0), (0, 0), (0, 0), (m_sub, 0))).reshape(B, G, Q_BLOCK, n_slc + 1, m_sub)
        for r in range(1, m_len):
            imp_slc = imp_slc + imp_pad[..., :n_slc, m_sub - r]
        cur = t // SLC_BLOCK
        valid_s = blk[None, :] <= cur[:, None]
        forced = (blk[None, :] == 0) | (blk[None, :] == cur[:, None]) | (blk[None, :] == cur[:, None] - 1)
        score = jnp.where(forced, FORCE, jnp.where(valid_s, imp_slc, NEG))
        _, sel = lax.top_k(score, n_sel)
        k_sel = _gather_blocks(k_slc, sel).reshape(B, G, Q_BLOCK, n_sel * SLC_BLOCK, DH)
        v_sel = _gather_blocks(v_slc, sel).reshape(B, G, Q_BLOCK, n_sel * SLC_BLOCK, DH)
        pos = (sel[..., None] * SLC_BLOCK + jnp.arange(SLC_BLOCK)).reshape(B, G, Q_BLOCK, n_sel * SLC_BLOCK)
        rel_s = t[None, None, :, None] - pos
        bias_s = jnp.moveaxis(tb[t5_bucket(rel_s), g_ix], -1, 2)
        s_s = jnp.einsum('btghd,bgtkd->bghtk', qb, k_sel).astype(jnp.float32) * scale
        s_s = jnp.where((rel_s >= 0)[:, :, None], s_s + bias_s, NEG)
        p_s = jax.nn.softmax(s_s, axis=-1)
        o_s = jnp.einsum('bghtk,bgtkd->btghd', p_s.astype(v_sel.dtype), v_sel)
        k_w = lax.dynamic_slice_in_dim(k_win, q0, Q_BLOCK + WINDOW, axis=1)
        v_w = lax.dynamic_slice_in_dim(v_win, q0, Q_BLOCK + WINDOW, axis=1)
        valid_w = (win_rel >= 0) & (win_rel < WINDOW) & ((q0 - WINDOW + k_loc) >= 0)[None, :]
        s_w = jnp.einsum('btghd,bkgd->bghtk', qb, k_w).astype(jnp.float32) * scale
        s_w = jnp.where(valid_w, s_w + win_bias, NEG)
        p_w = jax.nn.softmax(s_w, axis=-1)
        o_w = jnp.einsum('bghtk,bkgd->btghd', p_w.astype(v_w.dtype), v_w)
        o = gb[..., 0:1] * o_c + gb[..., 1:2] * o_s + gb[..., 2:3] * o_w
        return o.astype(h.dtype)

    o = lax.map(block_fn, (q_blocks, g_blocks, jnp.arange(nq)))
    o = jnp.moveaxis(o, 0, 1).reshape(B, S, N_HEADS * DH)
    return o @ w_out


def setup_inputs(seed: int = 0) -> dict:
    key = jax.random.key(seed)
    ks = iter(jax.random.split(key, 40))

    def nrm(shape, scale):
        return jax.random.normal(next(ks), shape, jnp.float32) * scale

    D = D_MODEL
    QW = N_HEADS * HEAD_DIM
    KVW = 2 * N_BRANCH * N_KV_GROUPS * HEAD_DIM
    return {
        "x": nrm((BATCH, SEQ, D), 1.0),
        "c": nrm((BATCH, D), 1.0),
        "mod_w": nrm((DEPTH, 2, D, 3 * D), D ** -0.5),
        "mod_b": nrm((DEPTH, 2, 3 * D), 0.01),
        "norm_pre": 1.0 + nrm((DEPTH, 2, D), 0.1),
        "norm_post": 1.0 + nrm((DEPTH, 2, D), 0.1),
        "a_w_in": nrm((N_A_LAYERS, D, 2 * A_WIDTH), D ** -0.5),
        "a_b_in": nrm((N_A_LAYERS, 2 * A_WIDTH), 0.01),
        "a_ln_g": 1.0 + nrm((N_A_LAYERS, A_WIDTH), 0.1),
        "a_ln_b": nrm((N_A_LAYERS, A_WIDTH), 0.01),
        "a_w_s": nrm((N_A_LAYERS, A_GROUPS, A_CHUNK, A_CHUNK), A_CHUNK ** -0.5),
        "a_b_s": 1.0 + nrm((N_A_LAYERS, A_GROUPS, A_CHUNK), 0.1),
        "a_w_out": nrm((N_A_LAYERS, A_WIDTH, D), A_WIDTH ** -0.5),
        "f_w_gate": nrm((DEPTH, D, D_FF), D ** -0.5),
        "f_w_up": nrm((DEPTH, D, D_FF), D ** -0.5),
        "f_conv_w": nrm((DEPTH, CONV_WIDTH, D_FF), CONV_WIDTH ** -0.5),
        "f_conv_b": nrm((DEPTH, D_FF), 0.01),
        "f_w_down": nrm((DEPTH, D_FF, D), D_FF ** -0.5),
        "kv_norm": 1.0 + nrm((D,), 0.1),
        "kv_mod_w": nrm((D, 2 * D), D ** -0.5),
        "kv_mod_b": nrm((2 * D,), 0.01),
        "w_kv": nrm((D, KVW), D ** -0.5),
        "cmp_pe": nrm((2, CMP_LEN, HEAD_DIM), 0.1),
        "cmp_w1": nrm((2, CMP_LEN * HEAD_DIM, HEAD_DIM), (CMP_LEN * HEAD_DIM) ** -0.5),
        "cmp_b1": nrm((2, HEAD_DIM), 0.01),
        "cmp_w2": nrm((2, HEAD_DIM, HEAD_DIM), HEAD_DIM ** -0.5),
        "b_w_in": nrm((N_B_LAYERS, D, QW + N_BRANCH * N_HEADS), D ** -0.5),
        "b_b_in": nrm((N_B_LAYERS, QW + N_BRANCH * N_HEADS), 0.01),
        "b_w_out": nrm((N_B_LAYERS, QW, D), QW ** -0.5),
        "rel_bias": nrm((N_BUCKETS, N_HEADS), 0.5),
    }


def reference(x, c, mod_w, mod_b, norm_pre, norm_post, a_w_in, a_b_in, a_ln_g, a_ln_b, a_w_s, a_b_s,
              a_w_out, f_w_gate, f_w_up, f_conv_w, f_conv_b, f_w_down, kv_norm, kv_mod_w, kv_mod_b,
              w_kv, cmp_pe, cmp_w1, cmp_b1, cmp_w2, b_w_in, b_b_in, b_w_out, rel_bias):
    c_act = jax.nn.silu(c)
    kvs = None
    for layer in range(DEPTH):
        if layer < N_A_LAYERS:
            i = layer
            mixer = lambda h, i=i: chunked_gmlp(h, a_w_in[i], a_b_in[i], a_ln_g[i], a_ln_b[i],
                                                a_w_s[i], a_b_s[i], a_w_out[i])
        else:
            if kvs is None:
                kvs = shared_kv(x, c_act, kv_norm, kv_mod_w, kv_mod_b, w_kv,
                                cmp_pe, cmp_w1, cmp_b1, cmp_w2)
            j = layer - N_A_LAYERS
            mixer = lambda h, j=j, kvs=kvs: nsa_attention(h, kvs, b_w_in[j], b_b_in[j], b_w_out[j], rel_bias)
        x = residual_sublayer(x, c_act, mod_w[layer, 0], mod_b[layer, 0],
                              norm_pre[layer, 0], norm_post[layer, 0], mixer)
        ffn = lambda h, l=layer: conv_ffn(h, f_w_gate[l], f_w_up[l], f_conv_w[l], f_conv_b[l], f_w_down[l])
        x = residual_sublayer(x, c_act, mod_w[layer, 1], mod_b[layer, 1],
                              norm_pre[layer, 1], norm_post[layer, 1], ffn)
    return x
```

```python
import numpy as np
import ml_dtypes
import concourse.bass as bass
import concourse.mybir as mybir
from concourse.bass_utils import run_bass_kernel_spmd

F32 = mybir.dt.float32
BF16 = mybir.dt.bfloat16
AF = mybir.ActivationFunctionType
ALU = mybir.AluOpType
AX = mybir.AxisListType

ENGS = ['pe', 'act', 'dve', 'pool', 'sp']
NDMA = {'sp': 20, 'pool': 20, 'act': 6}
SAME_ENGINE_SYNC = True
EPS = 1e-6


class Res:
    __slots__ = ('name', 'lw', 'rd')

    def __init__(self, name=''):
        self.name = name
        self.lw = None
        self.rd = {}


class Prog:
    def __init__(self, nc):
        self.nc = nc
        self.ops = {e: [] for e in ENGS}
        self.cnt = {e: 0 for e in ENGS}
        self.known = {e: {} for e in ENGS}
        self.dma_rr = {e: 0 for e in NDMA}
        self.dma_cnt = {}
        self.ctx = []
        self.nres = 0
        self.sems = {}
        self.guards = []
        for e in ENGS:
            g = nc.semaphore(f'es_{e}')
            self.sems[('E', e)] = g.__enter__()
            self.guards.append(g)
        for e, n in NDMA.items():
            for k in range(n):
                g = nc.semaphore(f'ds_{e}_{k}')
                self.sems[('D', (e, k))] = g.__enter__()
                self.guards.append(g)

    def sb(self, name, shape, dt):
        g = self.nc.sbuf_tensor("s_" + getattr(self, "pfx", "") + name, list(shape), dt)
        t = g.__enter__()
        self.ctx.append(g)
        return t

    def ps(self, name, shape, dt):
        g = self.nc.psum_tensor("p_" + name, list(shape), dt)
        t = g.__enter__()
        self.ctx.append(g)
        return t

    def res(self, name=''):
        self.nres += 1
        return Res(name or f'r{self.nres}')

    def op(self, eng, fn, reads=(), writes=(), dma=False):
        deps = []
        for r in reads:
            if r.lw is not None:
                deps.append(r.lw)
        for w in writes:
            if w.lw is not None:
                deps.append(w.lw)
            deps.extend(w.rd.values())
        if dma:
            k = self.dma_rr[eng]
            self.dma_rr[eng] = (k + 1) % NDMA[eng]
            key = ('D', (eng, k))
            prev = self.dma_cnt.get(key, 0)
            if prev > 0:
                deps.append(key + (prev,))
            self.dma_cnt[key] = prev + 16
            tok = key + (prev + 16,)
        else:
            self.cnt[eng] += 1
            tok = ('E', eng, self.cnt[eng])
        waits = {}
        kn = self.known[eng]
        for d in deps:
            key = d[:2]
            v = d[2]
            if key == ('E', eng):
                if eng == 'pe' or not SAME_ENGINE_SYNC:
                    continue
            if kn.get(key, 0) >= v:
                continue
            if waits.get(key, 0) < v:
                waits[key] = v
        for key, v in waits.items():
            kn[key] = v
        self.ops[eng].append((list(waits.items()), fn, tok))
        k2 = tok[:2]
        for r in reads:
            o = r.rd.get(k2)
            if o is None or o[2] < tok[2]:
                r.rd[k2] = tok
        for w in writes:
            w.lw = tok
            w.rd = {}
        return tok

    def dma(self, eng, out, in_, reads=(), writes=(), **kw):
        return self.op(eng, lambda e: e.dma_start(out=out, in_=in_, **kw), reads, writes, dma=True)

    def mm(self, out, lhsT, rhs, start, stop, reads, writes):
        return self.op('pe', lambda e: e.matmul(out, lhsT=lhsT, rhs=rhs, start=start, stop=stop), reads, writes)

    def tr(self, out, in_, ident, reads, writes):
        return self.op('pe', lambda e: e.transpose(out, in_, ident), reads, writes)

    def act(self, out, in_, func, reads, writes, eng='act', **kw):
        return self.op(eng, lambda e: e.activation(out=out, in_=in_, func=func, **kw), reads, writes)

    def ts(self, eng, out, in0, s1, s2, op0, op1, reads, writes, **kw):
        if op1 is None:
            return self.op(eng, lambda e: e.tensor_scalar(out=out, in0=in0, scalar1=s1, scalar2=None, op0=op0, **kw),
                           reads, writes)
        return self.op(eng, lambda e: e.tensor_scalar(out=out, in0=in0, scalar1=s1, scalar2=s2, op0=op0, op1=op1, **kw),
                       reads, writes)

    def tt(self, eng, out, in0, in1, op, reads, writes):
        return self.op(eng, lambda e: e.tensor_tensor(out=out, in0=in0, in1=in1, op=op), reads, writes)

    def stt(self, eng, out, in0, scalar, in1, op0, op1, reads, writes):
        return self.op(eng, lambda e: e.scalar_tensor_tensor(out=out, in0=in0, scalar=scalar, in1=in1, op0=op0, op1=op1),
                       reads, writes)

    def cp(self, eng, out, in_, reads, writes):
        if eng == 'act':
            return self.op(eng, lambda e: e.copy(out=out, in_=in_), reads, writes)
        return self.op(eng, lambda e: e.tensor_copy(out=out, in_=in_), reads, writes)

    def collective(self, kind, in_ap, out_ap, n_ranks, reads=(), writes=()):
        if not hasattr(self, 'cc_sem'):
            g = self.nc.semaphore('cc_sem')
            self.cc_sem = g.__enter__()
            self.guards.append(g)
            self.cc_cnt = 0
            self.sems[('C', 0)] = self.cc_sem
        deps = []
        for r in reads:
            if r.lw is not None:
                deps.append(r.lw)
        for w in writes:
            if w.lw is not None:
                deps.append(w.lw)
            deps.extend(w.rd.values())
        if self.cc_cnt > 0:
            deps.append(('C', 0, self.cc_cnt))
        self.cc_cnt += 1
        tok = ('C', 0, self.cc_cnt)
        waits = {}
        kn = self.known['pool']
        for d in deps:
            key, v = d[:2], d[2]
            if kn.get(key, 0) >= v:
                continue
            if waits.get(key, 0) < v:
                waits[key] = v
        for key, v in waits.items():
            kn[key] = v
        op = ALU.bypass
        fn = lambda e: e.collective_compute(kind, op, replica_groups=[list(range(n_ranks))], ins=[in_ap.opt()], outs=[out_ap.opt()])
        self.ops['pool'].append((list(waits.items()), fn, tok))
        for r in reads:
            r.rd[tok[:2]] = tok
        for w in writes:
            w.lw = tok
            w.rd = {}
        return tok

    def mark(self):
        return len(self.ctx)

    def barrier(self):
        snap_e = dict(self.cnt)
        snap_d = dict(self.dma_cnt)
        if getattr(self, 'cc_cnt', 0) > 0:
            snap_d[('C', 0)] = self.cc_cnt
        for e in ENGS:
            waits = []
            kn = self.known[e]
            for e2 in ENGS:
                key = ('E', e2)
                if e2 != e and snap_e[e2] > kn.get(key, 0):
                    waits.append((key, snap_e[e2]))
                    kn[key] = snap_e[e2]
            for key, v in snap_d.items():
                if v > kn.get(key, 0):
                    waits.append((key, v))
                    kn[key] = v
            self.cnt[e] += 1
            self.ops[e].append((waits, lambda eng: eng.nop(), ('E', e, self.cnt[e])))

    def flush(self, final=False):
        nc = self.nc
        sems = self.sems
        ops = self.ops
        fin = list(self.dma_cnt.items()) if final else []

        def run(eng_name, eng):
            for waits, fn, tok in ops[eng_name]:
                for key, v in waits:
                    eng.wait_ge(sems[key], v)
                ins = fn(eng)
                ins.then_inc(sems[tok[:2]], 16 if tok[0] == 'D' else 1)
            if eng_name == 'sp':
                if final and hasattr(self, 'cc_sem') and self.cc_cnt > 0:
                    eng.wait_ge(self.cc_sem, self.cc_cnt)
                for key, v in fin:
                    eng.wait_ge(sems[key], v)

        with nc.Block() as block:
            @block.tensor
            def _(e):
                run('pe', e)

            @block.scalar
            def _(e):
                run('act', e)

            @block.vector
            def _(e):
                run('dve', e)

            @block.gpsimd
            def _(e):
                run('pool', e)

            @block.sync
            def _(e):
                run('sp', e)
        self.ops = {e: [] for e in ENGS}

    def end_phase(self, mark):
        self.barrier()
        self.flush()
        Prog.last_sbuf_left = min(getattr(Prog, 'last_sbuf_left', 1 << 30), self.nc.sbuf_bytes_remaining)
        for g in reversed(self.ctx[mark:]):
            g.__exit__(None, None, None)
        del self.ctx[mark:]

    def emit(self):
        self.flush(final=True)
        Prog.last_sbuf_left = min(getattr(Prog, 'last_sbuf_left', 1 << 30), self.nc.sbuf_bytes_remaining)
        for g in reversed(self.guards):
            g.__exit__(None, None, None)
        for g in reversed(self.ctx):
            g.__exit__(None, None, None)


class Common:
    def __init__(self, P, nc, D):
        self.P = P
        self.D = D
        self.KC = D // 128
        ident_d = nc.dram_tensor("ident", [128, 128], F32, kind="ExternalInput").ap()
        self.nc = nc
        self.ident = P.sb("ident", [128, 128], F32)
        self.r_ident = P.res('ident')
        P.dma('sp', self.ident[:], ident_d, writes=[self.r_ident])
        self.ones = P.sb("ones", [128, 128], F32)
        self.r_ones = P.res('ones')
        P.op('dve', lambda e: e.memset(self.ones[:], 1.0), writes=[self.r_ones])
        self.onesb = P.sb("onesb", [128, 128], BF16)
        self.r_onesb = P.res('onesb')
        P.op('dve', lambda e: e.memset(self.onesb[:], 1.0), writes=[self.r_onesb])
        self.pb = [P.ps(f"pb{i}", [128, 512], F32) for i in range(8)]
        self.r_pb = [P.res(f'pb{i}') for i in range(8)]
        self.cnt = 0

    def bcast_row(self, row_ap, r_row, dst, r_dst, n, bank=0):
        P = self.P
        for j in range(0, n, 512):
            w = min(512, n - j)
            P.mm(self.pb[bank][:, 0:w], self.ones[0:1, :], row_ap[0:1, j:j + w], True, True,
                 [self.r_ones, r_row], [self.r_pb[bank]])
            P.cp('dve', dst[:, j:j + w], self.pb[bank][:, 0:w], [self.r_pb[bank]], [r_dst])


def rstd_from_ss(P, ss, rstd, r_ss, r_rstd, D):
    P.ts('dve', rstd, ss, 1.0 / D, EPS, ALU.mult, ALU.add, [r_ss], [r_rstd])
    P.act(rstd, rstd, AF.Sqrt, [r_rstd], [r_rstd])
    P.op('dve', lambda e: e.reciprocal(out=rstd, in_=rstd), [r_rstd], [r_rstd])


def prenorm_block(C, x_rows, npart, xt, r_xt, xn, r_xn, junk, r_junk, stat, r_stat, A, Sh, r_mod,
                  hT_dst, r_hT, banks, load_x=True):
    P = C.P
    D, KC = C.D, C.KC
    if load_x:
        if isinstance(x_rows, (list, tuple)):
            r0 = 0
            for piece in x_rows:
                n = piece.shape[0]
                P.dma('sp', xt[r0:r0 + n, :], piece, writes=[r_xt])
                r0 += n
        else:
            P.dma('sp', xt[0:npart, :], x_rows, writes=[r_xt])
    ss = stat[0:npart, 0:1]
    rs = stat[0:npart, 1:2]
    rj = list(r_junk) if isinstance(r_junk, (list, tuple)) else [r_junk]
    rm = list(r_mod) if isinstance(r_mod, (list, tuple)) else [r_mod]
    P.act(junk[0:npart, :], xt[0:npart, :], AF.Square, [r_xt], rj + [r_stat], accum_out=ss)
    rstd_from_ss(P, ss, rs, r_stat, r_stat, D)
    P.ts('dve', xn[0:npart, :], xt[0:npart, :], rs, None, ALU.mult, None, [r_xt, r_stat], [r_xn])
    per = 512 // npart if npart >= 4 else 128
    per = min(per, 128)
    kc = 0
    bi = 0
    while kc < KC:
        n = min(per, KC - kc)
        b = banks[bi % len(banks)]
        bi += 1
        for j in range(n):
            P.tr(C.pb[b][:, j * npart:(j + 1) * npart], xn[0:npart, (kc + j) * 128:(kc + j + 1) * 128],
                 C.ident[0:npart, 0:npart], [r_xn, C.r_ident], [C.r_pb[b]])
        for j in range(n):
            eng = 'act' if (j % 2 == 0) else 'dve'
            src = C.pb[b][:, j * npart:(j + 1) * npart]
            if eng == 'act':
                P.act(hT_dst(kc + j), src, AF.Identity, [C.r_pb[b]] + rm, [r_hT],
                      scale=A[:, kc + j:kc + j + 1], bias=Sh[:, kc + j:kc + j + 1])
            else:
                P.ts('dve', hT_dst(kc + j), src, A[:, kc + j:kc + j + 1], Sh[:, kc + j:kc + j + 1], ALU.mult, ALU.add,
                     [C.r_pb[b]] + rm, [r_hT])
        kc += n


def postnorm_block(C, y, r_y, x_rows, xt, r_xt, junk, r_junk, stat, r_stat, GP, r_GP, out_rows):
    P = C.P
    D = C.D
    P.dma('sp', xt[:, :], x_rows, writes=[r_xt])
    ss = stat[:, 2:3]
    rs = stat[:, 3:4]
    P.act(junk[:, :], y[:, :], AF.Square, [r_y], [r_junk, r_stat], accum_out=ss)
    rstd_from_ss(P, ss, rs, r_stat, r_stat, D)
    P.stt('dve', y[:, :], y[:, :], rs, GP[:, :], ALU.mult, ALU.mult, [r_y, r_stat, r_GP], [r_y])
    P.tt('dve', y[:, :], y[:, :], xt[:, :], ALU.add, [r_y, r_xt], [r_y])
    P.dma('sp', out_rows, y[:, :], reads=[r_y])


def load_mods(C, nc, pfx, scratch, r_scratch, fz=None):
    P = C.P
    D, KC = C.D, C.KC
    A = P.sb(pfx + "A", [128, KC], F32)
    GP = P.sb(pfx + "GP", [128, D], F32)
    r_mod, r_GP = P.res(), P.res()
    if fz is None:
        mcol_d = nc.dram_tensor(pfx + "mcol", [128, 3, KC], F32, kind="ExternalInput").ap()
        mrow_d = nc.dram_tensor(pfx + "mrow", [2, D], F32, kind="ExternalInput").ap()
        mcol = P.sb(pfx + "mcol", [128, 3, KC], F32)
        P.dma('sp', mcol[:], mcol_d, writes=[r_mod])
        P.dma('sp', GP[:, :], mrow_d[0:1, :].partition_broadcast(128), writes=[r_GP])
        P.dma('sp', scratch[:, :], mrow_d[1:2, :].partition_broadcast(128), writes=[r_scratch])
        P.stt('dve', A[:, :], mcol[:, 1, :], 1.0, mcol[:, 2, :], ALU.add, ALU.mult, [r_mod], [r_mod])
        P.tt('dve', GP[:, :], GP[:, :], scratch[:, :], ALU.mult, [r_GP, r_scratch], [r_GP])
        return A, mcol[:, 0, :], r_mod, GP, r_GP
    modT, r_modT, modg_d, r_modg, (i_sh, i_sc, i_gt), ncores = fz
    gpre_d = nc.dram_tensor(pfx + "gpre", [128, KC], F32, kind="ExternalInput").ap()
    gpre = P.sb(pfx + "gpre", [128, KC], F32)
    P.dma('sp', gpre[:], gpre_d, writes=[r_mod])
    P.stt('dve', A[:, :], modT[:, i_sc, :], 1.0, gpre[:, :], ALU.add, ALU.mult, [r_mod, r_modT], [r_mod])
    if i_gt is not None:
        gpost_d = nc.dram_tensor(pfx + "gpost", [1, D], F32, kind="ExternalInput").ap()
        w = D // ncores
        src = modg_d[:, i_gt * w:(i_gt + 1) * w]
        P.dma('sp', GP[:, :].rearrange("p (r k) -> p r k", r=ncores), src.unsqueeze(0).to_broadcast([128, ncores, w]),
              reads=[r_modg], writes=[r_GP])
        P.dma('sp', scratch[:, :], gpost_d[0:1, :].partition_broadcast(128), writes=[r_scratch])
        P.tt('dve', GP[:, :], GP[:, :], scratch[:, :], ALU.mult, [r_GP, r_scratch], [r_GP])
    return A, modT[:, i_sh, :], [r_mod, r_modT], GP, r_GP


def build_ffn(D, F, NT, TT=256, KS=8):
    nc = bass.Bass("TRN2", target_bir_lowering=False)
    KC, FC, NB, NBLK = D // 128, F // 128, TT // 128, NT // 128
    ND = D // 512 if D >= 512 else 1
    DW = min(D, 512)
    x_d = nc.dram_tensor("x", [NT, D], F32, kind="ExternalInput").ap()
    xh_d = nc.dram_tensor("xh", [NBLK * 2, D], F32, kind="ExternalInput").ap()
    hm_d = nc.dram_tensor("hmask", [128, NBLK * 2], F32, kind="ExternalInput").ap()
    wg_d = nc.dram_tensor("w_gate", [D, F], F32, kind="ExternalInput").ap()
    wu_d = nc.dram_tensor("w_up", [D, F], F32, kind="ExternalInput").ap()
    wd_d = nc.dram_tensor("w_down", [F, D], F32, kind="ExternalInput").ap()
    cw_d = nc.dram_tensor("convc", [128, 4, FC], F32, kind="ExternalInput").ap()
    o_d = nc.dram_tensor("out", [NT, D], F32, kind="ExternalOutput").ap()
    P = Prog(nc)
    C = Common(P, nc, D)
    cw = P.sb("cw", [128, 4, FC], F32)
    r_cw = P.res()
    P.dma('sp', cw[:], cw_d, writes=[r_cw])
    hm = P.sb("hm", [128, NBLK * 2], F32)
    r_hm = P.res()
    P.dma('sp', hm[:], hm_d, writes=[r_hm])

    hT = [P.sb(f"hT{i}", [128, KC, TT], BF16) for i in range(2)]
    r_hT = [P.res(), P.res()]
    hh = [P.sb(f"hh{i}", [128, KC, 2 * NB], BF16) for i in range(2)]
    r_hh = [P.res(), P.res()]
    gT = P.sb("gT", [128, FC, TT], BF16)
    r_gT = [P.res() for _ in range(FC)]
    wg = [P.sb(f"wg{i}", [128, KC, 128], BF16) for i in range(2)]
    wu = [P.sb(f"wu{i}", [128, KC, 128], BF16) for i in range(2)]
    r_wg = [P.res(), P.res()]
    r_wu = [P.res(), P.res()]
    wd = [P.sb(f"wd{i}", [128, KS, DW], BF16) for i in range(2)]
    r_wd = [P.res(), P.res()]
    y = [P.sb(f"y{i}", [128, D], F32) for i in range(NB)]
    r_y = [P.res() for _ in range(NB)]
    A, Sh, r_mod, GP, r_GP = load_mods(C, nc, "f_", y[0], r_y[0])
    xt = P.sb("xt", [128, D], F32)
    r_xt = P.res()
    junk = P.sb("junk", [128, D], BF16)
    r_junk = P.res()
    stat = P.sb("stat", [128, 8], F32)
    r_stat = P.res()
    asb = P.sb("asb", [128, NB, 130], F32)
    r_asb = P.res()
    acc = P.sb("acc", [128, NB, 128], F32)
    r_acc = P.res()
    hhf = P.sb("hhf", [128, KC, 2 * NB], F32)
    r_hhf = P.res()

    wgv = wg_d.rearrange("(kc p) f -> p kc f", p=128)
    wuv = wu_d.rearrange("(kc p) f -> p kc f", p=128)
    wdv = wd_d.rearrange("(fc p) n -> p fc n", p=128)
    nslab = 0
    ndslab = 0
    for ti in range(NT // TT):
        s = ti % 2
        for b in range(NB):
            blk = ti * NB + b
            prenorm_block(C, x_d[blk * 128:(blk + 1) * 128, :], 128, xt, r_xt, y[0], r_y[0], junk, r_junk, stat, r_stat,
                          A, Sh, r_mod, (lambda kc, b=b: hT[s][:, kc, b * 128:(b + 1) * 128]), r_hT[s], [0, 1])
        nh = 2 * NB
        prenorm_block(C, xh_d[ti * nh:(ti + 1) * nh, :], nh, xt, r_xt, y[0], r_y[0], junk, r_junk, stat, r_stat,
                      A, Sh, r_mod, (lambda kc: hhf[:, kc, :]), r_hhf, [0, 1])
        P.tt('dve', hh[s][:, :, :], hhf[:, :, :], hm[:, ti * nh:(ti + 1) * nh].unsqueeze(1).to_broadcast([128, KC, nh]),
             ALU.mult, [r_hhf, r_hm], [r_hh[s]])
        for fc in range(FC):
            ws = nslab % 2
            nslab += 1
            P.dma('pool', wg[ws][:], wgv[:, :, fc * 128:(fc + 1) * 128], writes=[r_wg[ws]])
            P.dma('pool', wu[ws][:], wuv[:, :, fc * 128:(fc + 1) * 128], writes=[r_wu[ws]])
            ba = 2 + ws
            bh = 4 + ws
            for kc in range(KC):
                P.mm(C.pb[ba][:, 0:TT], wg[ws][:, kc, :], hT[s][:, kc, :], kc == 0, kc == KC - 1,
                     [r_wg[ws], r_hT[s]], [C.r_pb[ba]])
            for kc in range(KC):
                P.mm(C.pb[bh][:, 0:nh], wg[ws][:, kc, :], hh[s][:, kc, :], kc == 0, kc == KC - 1,
                     [r_wg[ws], r_hh[s]], [C.r_pb[bh]])
            for kc in range(KC):
                P.mm(C.pb[ba][:, 256:256 + TT], wu[ws][:, kc, :], hT[s][:, kc, :], kc == 0, kc == KC - 1,
                     [r_wu[ws], r_hT[s]], [C.r_pb[ba]])
            P.cp('act', asb[:, :, 2:130], C.pb[ba][:, 0:TT].rearrange("p (b t) -> p b t", b=NB), [C.r_pb[ba]], [r_asb])
            P.cp('act', asb[:, :, 0:2], C.pb[bh][:, 0:nh].rearrange("p (b t) -> p b t", b=NB), [C.r_pb[bh]], [r_asb])
            P.ts('dve', acc[:, :, :], asb[:, :, 2:130], cw[:, 2, fc:fc + 1], cw[:, 3, fc:fc + 1], ALU.mult, ALU.add,
                 [r_asb, r_cw], [r_acc])
            P.stt('dve', acc[:, :, :], asb[:, :, 1:129], cw[:, 1, fc:fc + 1], acc[:, :, :], ALU.mult, ALU.add,
                  [r_asb, r_cw, r_acc], [r_acc])
            P.stt('dve', acc[:, :, :], asb[:, :, 0:128], cw[:, 0, fc:fc + 1], acc[:, :, :], ALU.mult, ALU.add,
                  [r_asb, r_cw, r_acc], [r_acc])
            P.act(acc[:, :, :], acc[:, :, :], AF.Silu, [r_acc], [r_acc])
            P.tt('dve', gT[:, fc, :].rearrange("p (b t) -> p b t", b=NB), acc[:, :, :],
                 C.pb[ba][:, 256:256 + TT].rearrange("p (b t) -> p b t", b=NB), ALU.mult,
                 [r_acc, C.r_pb[ba]], [r_gT[fc]])
        for n in range(ND):
            for k0 in range(0, FC, KS):
                kn = min(KS, FC - k0)
                ds = ndslab % 2
                ndslab += 1
                P.dma('pool', wd[ds][:, 0:kn, :], wdv[:, k0:k0 + kn, n * DW:(n + 1) * DW], writes=[r_wd[ds]])
                for b in range(NB):
                    for k in range(kn):
                        fc = k0 + k
                        P.mm(C.pb[6 + b][:, 0:DW], gT[:, fc, b * 128:(b + 1) * 128], wd[ds][:, k, :], fc == 0, fc == FC - 1,
                             [r_gT[fc], r_wd[ds]], [C.r_pb[6 + b]])
            for b in range(NB):
                P.cp('act', y[b][:, n * DW:(n + 1) * DW], C.pb[6 + b][:, 0:DW], [C.r_pb[6 + b]], [r_y[b]])
        for b in range(NB):
            blk = ti * NB + b
            postnorm_block(C, y[b], r_y[b], x_d[blk * 128:(blk + 1) * 128, :], xt, r_xt, junk, r_junk, stat, r_stat,
                           GP, r_GP, o_d[blk * 128:(blk + 1) * 128, :])
    P.emit()
    return nc


def build_ffn2(D, F, NT, TT=512, KS=4, ctx=None, io=None, pfx="", fz=None, halo_src=None):
    own = ctx is None
    if own:
        nc = bass.Bass("TRN2", target_bir_lowering=False)
        P = Prog(nc)
        C = Common(P, nc, D)
    else:
        nc, P, C = ctx
    io = io or {}
    P.pfx = pfx

    def dt(name, shape, ty, kind="ExternalInput"):
        if name in io:
            return io[name]
        return nc.dram_tensor(pfx + name, list(shape), ty, kind=kind).ap()
    m_phase = P.mark()
    KC, FC, NB, NBLK = D // 128, F // 128, TT // 128, NT // 128
    DW = min(D, 512)
    ND = D // DW
    HW = D // 2
    x_d = dt("x", [NT, D], F32)
    xh_d = dt("xh", [NBLK * 2, D], F32) if halo_src is None else None
    hm_d = dt("hmask", [128, NBLK * 2], F32)
    wg_d = dt("w_gate", [FC, 128, KC, 128], F32)
    wu_d = dt("w_up", [FC, 128, KC, 128], F32)
    wd_d = dt("w_down", [F, D], F32)
    cw_d = dt("convc", [128, 4, FC], F32)
    o_d = dt("out", [NT, D], F32, kind="ExternalOutput")
    ys_d = dt("yscr", [NT, D], F32, kind="Internal")
    r_ys = [Res() for _ in range(NBLK)]
    xt = P.sb("xt", [128, D], F32)
    r_xt = P.res()
    A, Sh, r_mod, GP, r_GP = load_mods(C, nc, pfx + "f_", xt, r_xt, fz)
    cw = P.sb("cw", [128, 4, FC], F32)
    r_cw = P.res()
    P.dma('sp', cw[:], cw_d, writes=[r_cw])
    hm = P.sb("hm", [128, NBLK * 2], F32)
    r_hm = P.res()
    P.dma('sp', hm[:], hm_d, writes=[r_hm])
    hT = P.sb("hT", [128, KC, TT], BF16)
    r_hT = P.res()
    hh = P.sb("hh", [128, KC, 2 * NB], BF16)
    r_hh = P.res()
    gT = P.sb("gT", [128, FC, TT], BF16)
    r_gT = [P.res() for _ in range(FC)]
    nj = (D + TT - 1) // TT
    junk = gT[:, 0:nj, :].rearrange("p a b -> p (a b)")[:, 0:D]
    r_junk = r_gT[0:nj]
    wg = [P.sb(f"wg{i}", [128, KC, 128], BF16) for i in range(2)]
    wu = [P.sb(f"wu{i}", [128, KC, 128], BF16) for i in range(2)]
    r_wg = [P.res(), P.res()]
    r_wu = [P.res(), P.res()]
    wd = [P.sb(f"wd{i}", [128, KS, DW], BF16) for i in range(2)]
    r_wd = [P.res(), P.res()]
    yst = [P.sb(f"yst{i}", [128, DW], F32) for i in range(2)]
    r_yst = [P.res(), P.res()]
    jk2 = P.sb("jk2", [128, DW], BF16)
    r_jk2 = P.res()
    stat = P.sb("stat", [128, 8], F32)
    r_stat = P.res()
    ssq = P.sb("ssq", [128, NB, ND], F32)
    r_ssq = P.res()
    asb = P.sb("asb", [128, NB, 130], F32)
    r_asb = P.res()
    acc = P.sb("acc", [128, NB, 128], F32)
    r_acc = P.res()
    hhf = P.sb("hhf", [128, KC, 2 * NB], F32)
    r_hhf = P.res()
    wdv = wd_d.rearrange("(fc p) n -> p fc n", p=128)
    nslab = ndslab = nyst = 0
    nh = 2 * NB
    for ti in range(NT // TT):
        for b in range(NB):
            blk = ti * NB + b
            prenorm_block(C, x_d[blk * 128:(blk + 1) * 128, :], 128, xt, r_xt, xt, r_xt, junk, r_junk, stat, r_stat,
                          A, Sh, r_mod, (lambda kc, b=b: hT[:, kc, b * 128:(b + 1) * 128]), r_hT, [0, 1])
        hsrc = xh_d[ti * nh:(ti + 1) * nh, :] if halo_src is None else [halo_src(ti * NB + b) for b in range(NB)]
        prenorm_block(C, hsrc, nh, xt, r_xt, xt, r_xt, junk, r_junk, stat, r_stat,
                      A, Sh, r_mod, (lambda kc: hhf[:, kc, :]), r_hhf, [0, 1])
        P.tt('dve', hh[:, :, :], hhf[:, :, :], hm[:, ti * nh:(ti + 1) * nh].unsqueeze(1).to_broadcast([128, KC, nh]),
             ALU.mult, [r_hhf, r_hm], [r_hh])
        for fc in range(FC):
            ws = nslab % 2
            nslab += 1
            P.dma('pool', wg[ws][:], wg_d[fc], writes=[r_wg[ws]])
            P.dma('pool', wu[ws][:], wu_d[fc], writes=[r_wu[ws]])
            ba, bu, bh = ws, 2 + ws, 4 + ws
            for kc in range(KC):
                P.mm(C.pb[ba][:, 0:TT], wg[ws][:, kc, :], hT[:, kc, :], kc == 0, kc == KC - 1, [r_wg[ws], r_hT], [C.r_pb[ba]])
            for kc in range(KC):
                P.mm(C.pb[bh][:, 0:nh], wg[ws][:, kc, :], hh[:, kc, :], kc == 0, kc == KC - 1, [r_wg[ws], r_hh], [C.r_pb[bh]])
            for kc in range(KC):
                P.mm(C.pb[bu][:, 0:TT], wu[ws][:, kc, :], hT[:, kc, :], kc == 0, kc == KC - 1, [r_wu[ws], r_hT], [C.r_pb[bu]])
            P.cp('act', asb[:, :, 2:130], C.pb[ba][:, 0:TT].rearrange("p (b t) -> p b t", b=NB), [C.r_pb[ba]], [r_asb])
            P.cp('act', asb[:, :, 0:2], C.pb[bh][:, 0:nh].rearrange("p (b t) -> p b t", b=NB), [C.r_pb[bh]], [r_asb])
            P.ts('dve', acc[:, :, :], asb[:, :, 2:130], cw[:, 2, fc:fc + 1], cw[:, 3, fc:fc + 1], ALU.mult, ALU.add,
                 [r_asb, r_cw], [r_acc])
            P.stt('dve', acc[:, :, :], asb[:, :, 1:129], cw[:, 1, fc:fc + 1], acc[:, :, :], ALU.mult, ALU.add,
                  [r_asb, r_cw, r_acc], [r_acc])
            P.stt('dve', acc[:, :, :], asb[:, :, 0:128], cw[:, 0, fc:fc + 1], acc[:, :, :], ALU.mult, ALU.add,
                  [r_asb, r_cw, r_acc], [r_acc])
            P.act(acc[:, :, :], acc[:, :, :], AF.Silu, [r_acc], [r_acc])
            P.tt('dve', gT[:, fc, :].rearrange("p (b t) -> p b t", b=NB), acc[:, :, :],
                 C.pb[bu][:, 0:TT].rearrange("p (b t) -> p b t", b=NB), ALU.mult, [r_acc, C.r_pb[bu]], [r_gT[fc]])
        for n in range(ND):
            for k0 in range(0, FC, KS):
                kn = min(KS, FC - k0)
                ds = ndslab % 2
                ndslab += 1
                P.dma('pool', wd[ds][:, 0:kn, :], wdv[:, k0:k0 + kn, n * DW:(n + 1) * DW], writes=[r_wd[ds]])
                for b in range(NB):
                    for k in range(kn):
                        fc = k0 + k
                        P.mm(C.pb[4 + b][:, 0:DW], gT[:, fc, b * 128:(b + 1) * 128], wd[ds][:, k, :], fc == 0, fc == FC - 1,
                             [r_gT[fc], r_wd[ds]], [C.r_pb[4 + b]])
            for b in range(NB):
                blk = ti * NB + b
                ys = nyst % 2
                nyst += 1
                P.cp('act', yst[ys][:, :], C.pb[4 + b][:, 0:DW], [C.r_pb[4 + b]], [r_yst[ys]])
                P.act(jk2[:, :], yst[ys][:, :], AF.Square, [r_yst[ys]], [r_jk2, r_ssq], accum_out=ssq[:, b, n:n + 1])
                P.dma('sp', ys_d[blk * 128:(blk + 1) * 128, n * DW:(n + 1) * DW], yst[ys][:, :], reads=[r_yst[ys]], writes=[r_ys[blk]])
        for b in range(NB):
            blk = ti * NB + b
            ss, rs = stat[:, 2:3], stat[:, 3:4]
            P.op('dve', lambda e, b=b: e.tensor_reduce(out=ss, in_=ssq[:, b, :], axis=AX.X, op=ALU.add), [r_ssq], [r_stat])
            rstd_from_ss(P, ss, rs, r_stat, r_stat, D)
            for hf in range(2):
                csl = slice(hf * HW, (hf + 1) * HW)
                P.dma('sp', xt[:, 0:HW], ys_d[blk * 128:(blk + 1) * 128, csl], reads=[r_ys[blk]], writes=[r_xt])
                P.dma('sp', xt[:, HW:D], x_d[blk * 128:(blk + 1) * 128, csl], writes=[r_xt])
                P.stt('dve', xt[:, 0:HW], xt[:, 0:HW], rs, GP[:, csl], ALU.mult, ALU.mult, [r_xt, r_stat, r_GP], [r_xt])
                P.tt('dve', xt[:, 0:HW], xt[:, 0:HW], xt[:, HW:D], ALU.add, [r_xt], [r_xt])
                P.dma('sp', o_d[blk * 128:(blk + 1) * 128, csl], xt[:, 0:HW], reads=[r_xt])
    if own:
        P.emit()
        return nc
    P.end_phase(m_phase)
    return None


def cols(v):
    v = np.asarray(v, np.float32)
    return np.ascontiguousarray(v.reshape(-1, 128).T)


GELU = AF.Gelu_apprx_tanh


def build_gmlp(D, NT, TT=256, KS=8, ctx=None, io=None, pfx="", fz=None):
    own = ctx is None
    if own:
        nc = bass.Bass("TRN2", target_bir_lowering=False)
        P = Prog(nc)
        C = Common(P, nc, D)
    else:
        nc, P, C = ctx
    io = io or {}
    P.pfx = pfx

    def dt(name, shape, ty, kind="ExternalInput"):
        if name in io:
            return io[name]
        return nc.dram_tensor(pfx + name, list(shape), ty, kind=kind).ap()
    m_phase = P.mark()
    W = D
    KC, NB, NG = D // 128, TT // 128, W // 128
    DW = min(D, 512)
    ND = D // DW
    x_d = dt("x", [NT, D], F32)
    UW = 256 if W % 256 == 0 else 128
    winu_d = dt("a_w_in_u", [W // UW, 128, KC, UW], F32)
    win_d = dt("a_w_in_v", [D, W], F32)
    binc_d = dt("a_b_in_c", [128, NG], F32)
    binr_d = dt("a_b_in_r", [1, W], F32)
    lnc_d = dt("a_ln_c", [128, 2, NG], F32)
    ws_d = dt("a_w_s", [NG, 128, 128], F32)
    bs_d = dt("a_b_s", [1, NG * 128], F32)
    wout_d = dt("a_w_out", [W, D], F32)
    triu_d = dt("triu", [128, 128], F32)
    o_d = dt("out", [NT, D], F32, kind="ExternalOutput")
    big = [P.sb(f"big{i}", [128, W], F32) for i in range(NB)]
    r_big = [P.res() for _ in range(NB)]
    A, Sh, r_mod, GP, r_GP = load_mods(C, nc, pfx + "m_", big[0], r_big[0], fz)
    hT = P.sb("hT", [128, KC, TT], BF16)
    r_hT = P.res()
    uT = P.sb("uT", [128, NG, TT], BF16)
    r_uT = [P.res() for _ in range(NG)]
    vln = [P.sb(f"vln{i}", [128, W], BF16) for i in range(NB)]
    r_vln = [P.res() for _ in range(NB)]
    xt = P.sb("xt", [128, D], F32)
    r_xt = P.res()
    stat = P.sb("stat", [128, 8], F32)
    r_stat = P.res()
    CB = P.sb("CB", [128, NG, 128], F32)
    r_CB = P.res()
    wsT = P.sb("wsT", [128, NG, 128], BF16)
    r_wsT = P.res()
    wu = [P.sb(f"wu{i}", [128, KC, UW], BF16) for i in range(2)]
    r_wu = [P.res(), P.res()]
    wm = [P.sb(f"wm{i}", [128, KS, DW], BF16) for i in range(2)]
    r_wm = [P.res(), P.res()]
    binc = P.sb("binc", [128, NG], F32)
    lnc = P.sb("lnc", [128, 2, NG], F32)
    binr = P.sb("binr", [1, W], BF16)
    triu = P.sb("triu", [128, 128], F32)
    tmp = P.sb("tmp", [128, NB, 128], F32)
    r_tmp = P.res()
    r_cst = P.res()
    P.dma('sp', binc[:], binc_d, writes=[r_cst])
    P.dma('sp', lnc[:], lnc_d, writes=[r_cst])
    P.dma('sp', triu[:], triu_d, writes=[r_cst])
    P.dma('pool', binr[:], binr_d, writes=[r_cst])
    wsn = big[1] if NB > 1 else xt
    r_wsn = r_big[1] if NB > 1 else r_xt
    P.dma('sp', wsn[:, 0:NG * 128].rearrange("p (g s) -> p g s", g=NG), ws_d.rearrange("g t s -> t g s"), writes=[r_wsn])
    for g0 in range(0, NG, 4):
        gn = min(4, NG - g0)
        for j in range(gn):
            P.tr(C.pb[0][:, j * 128:(j + 1) * 128], wsn[:, (g0 + j) * 128:(g0 + j + 1) * 128], C.ident[:, :],
                 [r_wsn, C.r_ident], [C.r_pb[0]])
        P.tt('dve', wsT[:, g0:g0 + gn, :], C.pb[0][:, 0:gn * 128].rearrange("p (g t) -> p g t", g=gn),
             triu[:, :].unsqueeze(1).to_broadcast([128, gn, 128]), ALU.mult, [C.r_pb[0], r_cst], [r_wsT])
    P.dma('sp', big[0][:, 0:NG * 128], bs_d[0:1, :].partition_broadcast(128), writes=[r_big[0]])
    for g0 in range(0, NG, 4):
        gn = min(4, NG - g0)
        for j in range(gn):
            P.mm(C.pb[1][:, j * 128:(j + 1) * 128], C.onesb[:, :], wsT[:, g0 + j, :], True, True,
                 [C.r_onesb, r_wsT], [C.r_pb[1]])
        for j in range(gn):
            g = g0 + j
            P.stt('dve', CB[:, g, :], C.pb[1][:, j * 128:(j + 1) * 128], lnc[:, 1, g:g + 1], big[0][:, g * 128:(g + 1) * 128],
                  ALU.mult, ALU.add, [C.r_pb[1], r_cst, r_big[0]], [r_CB])

    winv = win_d.rearrange("(kc p) f -> p kc f", p=128)
    woutv = wout_d.rearrange("(kc p) n -> p kc n", p=128)
    nsl = 0
    nms = 0
    for ti in range(NT // TT):
        for b in range(NB):
            blk = ti * NB + b
            prenorm_block(C, x_d[blk * 128:(blk + 1) * 128, :], 128, xt, r_xt, big[0], r_big[0], vln[0], r_vln[0], stat, r_stat,
                          A, Sh, r_mod, (lambda kc, b=b: hT[:, kc, b * 128:(b + 1) * 128]), r_hT, [0, 1])
        for f0 in range(0, NG, UW // 128):
            ws = nsl % 2
            nsl += 1
            P.dma('pool', wu[ws][:], winu_d[f0 // (UW // 128)], writes=[r_wu[ws]])
            for fi in range(UW // 128):
                fc = f0 + fi
                bk = 2 + fc % 2
                for kc in range(KC):
                    P.mm(C.pb[bk][:, 0:TT], wu[ws][:, kc, fi * 128:(fi + 1) * 128], hT[:, kc, :], kc == 0, kc == KC - 1,
                         [r_wu[ws], r_hT], [C.r_pb[bk]])
                P.act(uT[:, fc, :], C.pb[bk][:, 0:TT], GELU, [C.r_pb[bk], r_cst], [r_uT[fc]], bias=binc[:, fc:fc + 1])
        for n in range(ND):
            for b in range(NB):
                P.mm(C.pb[6 + b][:, 0:DW], C.onesb[0:1, :], binr[0:1, n * DW:(n + 1) * DW], True, False,
                     [C.r_onesb, r_cst], [C.r_pb[6 + b]])
            for k0 in range(0, KC, KS):
                kn = min(KS, KC - k0)
                ms = nms % 2
                nms += 1
                P.dma('pool', wm[ms][:, 0:kn, :], winv[:, k0:k0 + kn, n * DW:(n + 1) * DW], writes=[r_wm[ms]])
                for b in range(NB):
                    for k in range(kn):
                        kc = k0 + k
                        P.mm(C.pb[6 + b][:, 0:DW], hT[:, kc, b * 128:(b + 1) * 128], wm[ms][:, k, :], False, kc == KC - 1,
                             [r_hT, r_wm[ms]], [C.r_pb[6 + b]])
            for b in range(NB):
                P.act(big[b][:, n * DW:(n + 1) * DW], C.pb[6 + b][:, 0:DW], GELU, [C.r_pb[6 + b]], [r_big[b]])
        for b in range(NB):
            s1, s2, mu, rs = stat[:, 4:5], stat[:, 5:6], stat[:, 6:7], stat[:, 7:8]
            P.op('dve', lambda e, b=b: e.tensor_reduce(out=s1, in_=big[b][:, :], axis=AX.X, op=ALU.add), [r_big[b]], [r_stat])
            P.act(vln[b][:, :], big[b][:, :], AF.Square, [r_big[b]], [r_vln[b], r_stat], accum_out=s2)
            P.ts('dve', mu, s1, 1.0 / W, None, ALU.mult, None, [r_stat], [r_stat])
            P.stt('dve', s1, mu, mu, mu, ALU.mult, ALU.subtract, [r_stat], [r_stat])
            P.tt('dve', s1, s1, mu, ALU.add, [r_stat], [r_stat])
            P.stt('dve', rs, s2, 1.0 / W, s1, ALU.mult, ALU.subtract, [r_stat], [r_stat])
            P.ts('dve', rs, rs, EPS, None, ALU.add, None, [r_stat], [r_stat])
            P.act(rs, rs, AF.Sqrt, [r_stat], [r_stat])
            P.op('dve', lambda e: e.reciprocal(out=rs, in_=rs), [r_stat], [r_stat])
            P.ts('dve', vln[b][:, :], big[b][:, :], mu, rs, ALU.subtract, ALU.mult, [r_big[b], r_stat], [r_vln[b]])
        for g in range(NG):
            bk = 4 + g % 2
            for b in range(NB):
                P.mm(C.pb[bk][:, b * 128:(b + 1) * 128], vln[b][:, g * 128:(g + 1) * 128], wsT[:, g, :], True, True,
                     [r_vln[b], r_wsT], [C.r_pb[bk]])
            P.stt('dve', tmp[:, :, :], C.pb[bk][:, 0:TT].rearrange("p (b t) -> p b t", b=NB), lnc[:, 0, g:g + 1],
                  CB[:, g, :].unsqueeze(1).to_broadcast([128, NB, 128]), ALU.mult, ALU.add, [C.r_pb[bk], r_cst, r_CB], [r_tmp])
            P.tt('dve', uT[:, g, :].rearrange("p (b t) -> p b t", b=NB), tmp[:, :, :],
                 uT[:, g, :].rearrange("p (b t) -> p b t", b=NB), ALU.mult, [r_tmp, r_uT[g]], [r_uT[g]])
        for n in range(ND):
            for k0 in range(0, NG, KS):
                kn = min(KS, NG - k0)
                ms = nms % 2
                nms += 1
                P.dma('pool', wm[ms][:, 0:kn, :], woutv[:, k0:k0 + kn, n * DW:(n + 1) * DW], writes=[r_wm[ms]])
                for b in range(NB):
                    for k in range(kn):
                        kc = k0 + k
                        P.mm(C.pb[6 + b][:, 0:DW], uT[:, kc, b * 128:(b + 1) * 128], wm[ms][:, k, :], kc == 0, kc == NG - 1,
                             [r_uT[kc], r_wm[ms]], [C.r_pb[6 + b]])
            for b in range(NB):
                P.cp('act', big[b][:, n * DW:(n + 1) * DW], C.pb[6 + b][:, 0:DW], [C.r_pb[6 + b]], [r_big[b]])
        for b in range(NB):
            blk = ti * NB + b
            postnorm_block(C, big[b], r_big[b], x_d[blk * 128:(blk + 1) * 128, :], xt, r_xt, vln[0], r_vln[0], stat, r_stat,
                           GP, r_GP, o_d[blk * 128:(blk + 1) * 128, :])
    if own:
        P.emit()
        return nc
    P.end_phase(m_phase)
    return None


def build_gemv(D, NCOL, ctx=None, io=None, pfx=""):
    own = ctx is None
    if own:
        nc = bass.Bass("TRN2", target_bir_lowering=False)
        P = Prog(nc)
        C = Common(P, nc, D)
    else:
        nc, P, C = ctx
    io = io or {}
    P.pfx = pfx

    def dt(name, shape, ty, kind="ExternalInput"):
        if name in io:
            return io[name]
        return nc.dram_tensor(pfx + name, list(shape), ty, kind=kind).ap()
    m_phase = P.mark()
    KC = D // 128
    cT = dt("cT", [128, KC], F32)
    Wd = dt("W", [D, NCOL], F32)
    b = dt("b", [1, NCOL], F32)
    o = dt("o", [1, NCOL], F32, kind="ExternalOutput")
    c32 = P.sb("c32", [128, KC], F32)
    cb = P.sb("cb", [128, KC], BF16)
    bias = P.sb("bias", [1, NCOL], F32)
    osb = P.sb("osb", [1, NCOL], F32)
    wbuf = [P.sb(f"w{i}", [128, KC, 512], BF16) for i in range(2)]
    pbank = [C.pb[0], C.pb[1]]
    r_c32, r_cb, r_bias, r_osb = P.res(), P.res(), P.res(), P.res()
    r_w = [P.res(), P.res()]
    r_p = [C.r_pb[0], C.r_pb[1]]
    P.dma('sp', c32[:], cT, writes=[r_c32])
    P.dma('sp', bias[:], b, writes=[r_bias])
    P.act(cb[:], c32[:], AF.Silu, [r_c32], [r_cb])
    Wv = Wd.rearrange("(kc p) n -> p kc n", p=128)
    for j in range(NCOL // 512):
        s = j % 2
        P.dma('pool', wbuf[s][:], Wv[:, :, j * 512:(j + 1) * 512], writes=[r_w[s]])
        for kc in range(KC):
            P.mm(pbank[s][0:1, :], cb[:, kc:kc + 1], wbuf[s][:, kc, :], kc == 0, kc == KC - 1, [r_cb, r_w[s]], [r_p[s]])
        P.tt('dve', osb[0:1, j * 512:(j + 1) * 512], pbank[s][0:1, :], bias[0:1, j * 512:(j + 1) * 512], ALU.add,
             [r_p[s], r_bias], [r_osb])
    P.dma('sp', o, osb[:], reads=[r_osb])
    if own:
        P.emit()
        return nc
    P.end_phase(m_phase)
    return None


def build_kv(D, G, NT, TT=256, KS=8, ctx=None, io=None, pfx="", fz=None):
    own = ctx is None
    if own:
        nc = bass.Bass("TRN2", target_bir_lowering=False)
        P = Prog(nc)
        C = Common(P, nc, D)
    else:
        nc, P, C = ctx
    io = io or {}
    P.pfx = pfx

    def dt(name, shape, ty, kind="ExternalInput"):
        if name in io:
            return io[name]
        return nc.dram_tensor(pfx + name, list(shape), ty, kind=kind).ap()
    m_phase = P.mark()
    KC, NB = D // 128, TT // 128
    GW = G * 128
    x_d = dt("x", [NT, D], F32)
    mcol_d = dt("kv_mcol", [128, 3, KC], F32) if fz is None else None
    wkv_d = dt("w_kv", [D, 6 * GW], F32)
    wkvk_d = dt("w_kv_k", [6 * G, 128, KC, 128], F32)
    kT_d = dt("kT", [128, 4, G, NT], BF16, kind="ExternalOutput")
    vt_d = dt("vtok", [NT, 2, GW], BF16, kind="ExternalOutput")
    A = P.sb("A", [128, KC], F32)
    r_mod = P.res()
    if fz is None:
        mcol = P.sb("mcol", [128, 3, KC], F32)
        P.dma('sp', mcol[:], mcol_d, writes=[r_mod])
        P.stt('dve', A[:, :], mcol[:, 1, :], 1.0, mcol[:, 2, :], ALU.add, ALU.mult, [r_mod], [r_mod])
        Sh = mcol[:, 0, :]
    else:
        modT, r_modT, _, _, (i_sh, i_sc, _), _ = fz
        kvn = P.sb("kvn", [128, KC], F32)
        P.dma('sp', kvn[:], dt("kvnorm", [128, KC], F32), writes=[r_mod])
        P.stt('dve', A[:, :], modT[:, i_sc, :], 1.0, kvn[:, :], ALU.add, ALU.mult, [r_mod, r_modT], [r_mod])
        Sh = modT[:, i_sh, :]
        r_mod = [r_mod, r_modT]
    hT = P.sb("hT", [128, KC, TT], BF16)
    r_hT = P.res()
    xt = P.sb("xt", [128, D], F32)
    xn = P.sb("xn", [128, D], F32)
    junk = P.sb("junk", [128, D], BF16)
    stat = P.sb("stat", [128, 8], F32)
    r_xt, r_xn, r_junk, r_stat = P.res(), P.res(), P.res(), P.res()
    wk = [P.sb(f"wk{i}", [128, KC, 128], BF16) for i in range(2)]
    r_wk = [P.res(), P.res()]
    wm = [P.sb(f"wm{i}", [128, KS, GW], BF16) for i in range(2)]
    r_wm = [P.res(), P.res()]
    kTs = [P.sb(f"kTs{i}", [128, TT], BF16) for i in range(2)]
    r_kTs = [P.res(), P.res()]
    vts = [P.sb(f"vts{i}", [128, GW], BF16) for i in range(2)]
    r_vts = [P.res(), P.res()]
    wv = wkv_d.rearrange("(kc p) f -> p kc f", p=128)
    nsl = nms = nk = nv = 0
    for ti in range(NT // TT):
        for b in range(NB):
            blk = ti * NB + b
            prenorm_block(C, x_d[blk * 128:(blk + 1) * 128, :], 128, xt, r_xt, xn, r_xn, junk, r_junk, stat, r_stat,
                          A, Sh, r_mod, (lambda kc, b=b: hT[:, kc, b * 128:(b + 1) * 128]), r_hT, [0, 1])
        for ki, bsrc in enumerate([0, 1, 2, 4]):
            for g in range(G):
                fc = bsrc * G + g
                ws = nsl % 2
                nsl += 1
                P.dma('pool', wk[ws][:], wkvk_d[fc], writes=[r_wk[ws]])
                bk = 2 + ws
                for kc in range(KC):
                    P.mm(C.pb[bk][:, 0:TT], wk[ws][:, kc, :], hT[:, kc, :], kc == 0, kc == KC - 1, [r_wk[ws], r_hT], [C.r_pb[bk]])
                ks = nk % 2
                nk += 1
                P.cp('act', kTs[ks][:, :], C.pb[bk][:, 0:TT], [C.r_pb[bk]], [r_kTs[ks]])
                P.dma('sp', kT_d[:, ki, g, ti * TT:(ti + 1) * TT], kTs[ks][:, :], reads=[r_kTs[ks]])
        for vi, bsrc in enumerate([3, 5]):
            for k0 in range(0, KC, KS):
                kn = min(KS, KC - k0)
                ms = nms % 2
                nms += 1
                P.dma('pool', wm[ms][:, 0:kn, :], wv[:, k0:k0 + kn, bsrc * GW:(bsrc + 1) * GW], writes=[r_wm[ms]])
                for b in range(NB):
                    for k in range(kn):
                        kc = k0 + k
                        P.mm(C.pb[6 + b][:, 0:GW], hT[:, kc, b * 128:(b + 1) * 128], wm[ms][:, k, :], kc == 0, kc == KC - 1,
                             [r_hT, r_wm[ms]], [C.r_pb[6 + b]])
            for b in range(NB):
                blk = ti * NB + b
                vs = nv % 2
                nv += 1
                P.cp('act', vts[vs][:, :], C.pb[6 + b][:, 0:GW], [C.r_pb[6 + b]], [r_vts[vs]])
                P.dma('sp', vt_d[blk * 128:(blk + 1) * 128, vi, :], vts[vs][:, :], reads=[r_vts[vs]])
    if own:
        P.emit()
        return nc
    P.end_phase(m_phase)
    return None


MNEG = -30000.0


def build_attn(D, S, H, G, NQB, KS=8, ctx=None, io=None, pfx="", fz=None):
    own = ctx is None
    if own:
        nc = bass.Bass("TRN2", target_bir_lowering=False)
        P = Prog(nc)
        C = Common(P, nc, D)
    else:
        nc, P, C = ctx
    io = io or {}
    P.pfx = pfx

    def dt(name, shape, ty, kind="ExternalInput"):
        if name in io:
            return io[name]
        return nc.dram_tensor(pfx + name, list(shape), ty, kind=kind).ap()
    m_phase = P.mark()
    HG = H // G
    QW = H * 128
    KC = D // 128
    NBA = S // 128
    NCMP = S // 16
    NSB = S // 64
    NTC = NCMP + 8 * (NQB - 1)
    NCC = (NCMP + 127) // 128
    NT = NQB * 128
    NG3 = 3 * H
    DW = min(D, 512)
    ND = D // DW
    sc = 128.0 ** -0.5
    x_d = dt("x", [NT, D], F32)
    win_d = dt("b_w_in", [D, QW + NG3], F32)
    winq_d = dt("b_w_in_q", [H, 128, KC, 128], F32)
    bq_d = dt("b_bq", [128, H], F32)
    bg_d = dt("b_bg", [1, NG3], F32)
    wout_d = dt("b_w_out", [QW, D], F32)
    kT_d = dt("kTall", [128, 4, G, S], BF16)
    vt_d = dt("vtall", [S, 2, G * 128], BF16)
    peT_d = dt("cmp_peT", [128, 2, 32], F32)
    w1_d = dt("cmp_w1", [2, 32 * 128, 128], F32)
    b1_d = dt("cmp_b1c", [128, 2], F32)
    w2_d = dt("cmp_w2", [2, 128, 128], F32)
    tabT_d = dt("tabT", [128, 9, H, 128], F32)
    tabC_d = dt("tabC", [128, H, NTC], F32)
    c31_d = dt("c31", [128, H], F32)
    mD0_d = dt("maskD0", [128, 128], F32)
    mW4_d = dt("maskW4", [128, 128], F32)
    maskC_d = dt("maskC", [NQB, 128, NCMP], F32)
    selt_d = dt("seltab", [NQB, 128, 3, NSB], F32)
    wpad_d = dt("wpad", [128, NQB, 5], F32)
    Ex_d = dt("Ex", [NSB, NBA, 128], F32)
    o_d = dt("out", [NT, D], F32, kind="ExternalOutput")
    tabF_d = dt("tabF", [H // 4, 128, 9, 4, 128], BF16, kind="Internal")
    r_tabF = [Res() for _ in range(H // 4)]

    q_scr = dt("q_scr", [128, H, NT], BF16, kind="Internal")
    g_scr = dt("g_scr", [NT, NG3], F32, kind="Internal")
    o_scr = dt("o_scr", [NT, QW], F32, kind="Internal")
    TTP = min(512, NT)
    NBP = TTP // 128

    identb = P.sb("identb", [128, 128], BF16)
    r_identb = P.res()
    P.cp('dve', identb[:, :], C.ident[:, :], [C.r_ident], [r_identb])
    xt = P.sb("xt", [128, D], F32)
    r_xt = P.res()
    A, Sh, r_mod, GP, r_GP = load_mods(C, nc, pfx + "b_", xt, r_xt, fz)
    stat = P.sb("stat", [128, 8], F32)
    r_stat = P.res()
    winv = win_d.rearrange("(kc p) f -> p kc f", p=128)
    woutv = wout_d.rearrange("(kc p) n -> p kc n", p=128)

    import os as _os
    if _os.environ.get("CC_TEST"):
        ncr = int(_os.environ["CC_TEST"])
        cc_in = dt("cc_in", [128, 64], F32, kind="Internal")
        cc_out = dt("cc_out", [ncr * 128, 64], F32, kind="Internal")
        r_cc = Res()
        P.dma('sp', cc_in, C.ident[:, 0:64], reads=[C.r_ident], writes=[r_cc])
        P.collective("AllGather", cc_in, cc_out, ncr, reads=[r_cc], writes=[r_cc])
    m1 = P.mark()
    hT = P.sb("hT", [128, KC, TTP], BF16)
    r_hT = P.res()
    xn1 = P.sb("xn1", [128, D], F32)
    r_xn1 = P.res()
    junk1 = P.sb("junk1", [128, D], BF16)
    r_junk1 = P.res()
    wbuf = [P.sb(f"wbuf{i}", [128, KC * 128], BF16) for i in range(2)]
    r_wbuf = [P.res(), P.res()]
    wgt = P.sb("wgt", [128, KC, NG3], BF16)
    bq = P.sb("bq", [128, H], F32)
    bgr = P.sb("bgr", [1, NG3], BF16)
    qst = [P.sb(f"qst{i}", [128, TTP], BF16) for i in range(2)]
    r_qst = [P.res(), P.res()]
    gst = [P.sb(f"gst{i}", [128, NG3], F32) for i in range(2)]
    r_gst = [P.res(), P.res()]
    r_c1 = P.res()
    P.dma('pool', wgt[:], winv[:, :, QW:QW + NG3], writes=[r_c1])
    P.dma('sp', bq[:], bq_d, writes=[r_c1])
    P.dma('pool', bgr[:], bg_d, writes=[r_c1])
    P.ts('dve', bq[:, :], bq[:, :], sc, None, ALU.mult, None, [r_c1], [r_c1])
    nsl = nq = ngs = 0
    for ti in range(NT // TTP):
        for b in range(NBP):
            blk = ti * NBP + b
            prenorm_block(C, x_d[blk * 128:(blk + 1) * 128, :], 128, xt, r_xt, xn1, r_xn1, junk1, r_junk1, stat, r_stat,
                          A, Sh, r_mod, (lambda kc, b=b: hT[:, kc, b * 128:(b + 1) * 128]), r_hT, [0, 1])
        for h in range(H):
            ws = nsl % 2
            nsl += 1
            wv_ = wbuf[ws][:, :].rearrange("p (k f) -> p k f", k=KC)
            P.dma('pool', wv_, winq_d[h], writes=[r_wbuf[ws]])
            bk = 2 + ws
            for kc in range(KC):
                P.mm(C.pb[bk][:, 0:TTP], wv_[:, kc, :], hT[:, kc, :], kc == 0, kc == KC - 1, [r_wbuf[ws], r_hT], [C.r_pb[bk]])
            qs = nq % 2
            nq += 1
            P.act(qst[qs][:, :], C.pb[bk][:, 0:TTP], AF.Identity, [C.r_pb[bk], r_c1], [r_qst[qs]], scale=sc, bias=bq[:, h:h + 1])
            P.dma('sp', q_scr[:, h, ti * TTP:(ti + 1) * TTP], qst[qs][:, :], reads=[r_qst[qs]])
        for b in range(NBP):
            blk = ti * NBP + b
            bk = 4 + b % 2
            P.mm(C.pb[bk][:, 0:NG3], C.onesb[0:1, :], bgr[0:1, :], True, False, [C.r_onesb, r_c1], [C.r_pb[bk]])
            for kc in range(KC):
                P.mm(C.pb[bk][:, 0:NG3], hT[:, kc, b * 128:(b + 1) * 128], wgt[:, kc, :], False, kc == KC - 1, [r_hT, r_c1], [C.r_pb[bk]])
            gs = ngs % 2
            ngs += 1
            P.act(gst[gs][:, :], C.pb[bk][:, 0:NG3], AF.Sigmoid, [C.r_pb[bk]], [r_gst[gs]])
            P.dma('sp', g_scr[blk * 128:(blk + 1) * 128, :], gst[gs][:, :], reads=[r_gst[gs]])
    P.end_phase(m1)

    m2 = P.mark()
    ocat = P.sb("ocat", [128, QW], F32)
    r_ocat = P.res()
    qT = P.sb("qT", [128, H, 128], BF16)
    r_qT = P.res()
    gates = P.sb("gates", [128, NG3], F32)
    r_gates = P.res()
    w1buf = P.sb("w1buf", [128, 32 * 128], BF16)
    r_w1 = P.res()
    c31 = P.sb("c31", [128, H], F32)
    mD0 = P.sb("mD0", [128, 128], F32)
    mW4 = P.sb("mW4", [128, 128], F32)
    wpad = P.sb("wpad", [128, NQB, 5], F32)
    Ex = P.sb("Ex", [NSB, NBA, 128], BF16)
    r_cst = P.res()
    P.dma('sp', c31[:], c31_d, writes=[r_cst])
    P.dma('sp', mD0[:], mD0_d, writes=[r_cst])
    P.dma('sp', mW4[:], mW4_d, writes=[r_cst])
    P.dma('sp', wpad[:], wpad_d, writes=[r_cst])
    P.dma('pool', Ex[:], Ex_d, writes=[r_cst])
    kcT = P.sb("kcT", [128, G, NCC * 128], BF16)
    vc = P.sb("vc", [128, NCC, G, 128], BF16)
    r_kc = P.res()
    ksT = P.sb("ksT", [128, NBA * 128], BF16)
    vs = P.sb("vs", [128, NBA, 129], BF16)
    kwT = P.sb("kwT", [128, 5 * 128], BF16)
    vw = P.sb("vw", [128, 5, 129], BF16)
    r_ks, r_vs, r_kw, r_vw = P.res(), P.res(), P.res(), P.res()
    P.op('dve', lambda e: e.memset(vs[:, :, 128:129], 1.0), writes=[r_vs])
    P.op('dve', lambda e: e.memset(vw[:, :, 128:129], 1.0), writes=[r_vw])
    tabs = [P.sb(f"tab{i}", [128, 9, 4, 128], BF16) for i in range(2)]
    r_tabs = [P.res(), P.res()]
    tab, r_tab = tabs[0], r_tabs[0]
    tabtmp = P.sb("tabtmp", [128, 3, 4, 128], F32)
    r_tabtmp = P.res()
    tabC = [P.sb(f"tabC{i}", [128, NTC], F32) for i in range(2)]
    r_tabC = [P.res(), P.res()]
    maskC = P.sb("maskC", [128, NCMP], F32)
    selt = P.sb("selt", [128, 3, NSB], F32)
    r_slot = P.res()
    Ssb = P.sb("Ssb", [128, NCMP], F32)
    e32 = P.sb("e32", [128, NCC * 128], F32)
    eT = P.sb("eT", [128, NCC, 128], BF16)
    imp = P.sb("imp", [128, NCMP], F32)
    r_Ssb, r_e32, r_eT, r_imp = P.res(), P.res(), P.res(), P.res()
    scr = P.sb("scr", [128, 4, NSB], F32)
    r_scr = P.res()
    top8 = P.sb("top8", [128, 16], F32)
    sm = P.sb("sm", [128, 16], F32)
    r_sm = P.res()
    MaddT = P.sb("MaddT", [NSB, 128], BF16)
    r_MaddT = P.res()
    selexp = P.sb("selexp", [128, NBA, 128], BF16)
    r_selexp = P.res()
    ET = [P.sb(f"ET{i}", [128, 512], BF16) for i in range(2)]
    r_ET = [P.res(), P.res()]
    if NCC * 128 > NCMP:
        P.op('dve', lambda e: e.memset(e32[:, :], 0.0), writes=[r_e32])

    for hq4 in range(H // 4):
        h0 = hq4 * 4
        for part in range(3):
            P.dma('sp', tabtmp[:, :, :, :], tabT_d[:, part * 3:(part + 1) * 3, h0:h0 + 4, :], writes=[r_tabtmp])
            for q in range(4):
                P.ts('dve', tabtmp[:, :, q, :], tabtmp[:, :, q, :], c31[:, h0 + q:h0 + q + 1], None, ALU.subtract, None,
                     [r_tabtmp, r_cst], [r_tabtmp])
            if part == 0:
                P.tt('dve', tabtmp[:, 0, :, :], tabtmp[:, 0, :, :], mD0[:, :].unsqueeze(1).to_broadcast([128, 4, 128]), ALU.add,
                     [r_tabtmp, r_cst], [r_tabtmp])
            if part == 2:
                P.tt('dve', tabtmp[:, 2, :, :], tabtmp[:, 2, :, :], mW4[:, :].unsqueeze(1).to_broadcast([128, 4, 128]), ALU.add,
                     [r_tabtmp, r_cst], [r_tabtmp])
            P.cp('dve', tab[:, part * 3:(part + 1) * 3, :, :], tabtmp[:, :, :, :], [r_tabtmp], [r_tab])
        P.dma('sp', tabF_d[hq4], tab[:, :, :, :], reads=[r_tab], writes=[r_tabF[hq4]])

    rawT = ksT
    w1b = w1buf[:, :].rearrange("p (l j) -> p l j", l=32)
    peT = P.sb("peT", [128, 2, 32], BF16)
    b1c = P.sb("b1c", [128, 2], F32)
    w2b = P.sb("w2b", [128, 2, 128], BF16)
    hidT = P.sb("hidT", [128, NCC * 128], BF16)
    r_hid = P.res()
    P.dma('pool', peT[:], peT_d, writes=[r_cst])
    P.dma('sp', b1c[:], b1_d, writes=[r_cst])
    P.dma('pool', w2b[:], w2_d.rearrange("k j d -> j k d"), writes=[r_cst])
    P.op('dve', lambda e: e.memset(hidT[:, :], 0.0), writes=[r_hid])
    NF = NCMP - 1
    for kind in range(2):
        P.dma('pool', w1b, w1_d[kind].rearrange("(l d) j -> d l j", d=128), writes=[r_w1])
        for l in range(32):
            P.mm(C.pb[6][:, 0:1], w1b[:, l, :], peT[:, kind, l:l + 1], l == 0, l == 31, [r_w1, r_cst], [C.r_pb[6]])
        P.tt('dve', sm[:, 8 + kind:9 + kind], C.pb[6][:, 0:1], b1c[:, kind:kind + 1], ALU.add, [C.r_pb[6], r_cst], [r_sm])
        for g in range(G):
            P.dma('sp', rawT[:, 0:S], kT_d[:, kind, g, :], writes=[r_ks])
            for l in range(32):
                P.mm(C.pb[4][:, 0:NF], w1b[:, l, :], rawT[:, l:l + 16 * (NF - 1) + 1:16], l == 0, l == 31,
                     [r_w1, r_ks], [C.r_pb[4]])
            P.act(hidT[:, 0:NF], C.pb[4][:, 0:NF], GELU, [C.r_pb[4], r_sm], [r_hid], bias=sm[:, 8 + kind:9 + kind])
            if kind == 0:
                P.mm(C.pb[5][:, 0:NCC * 128], w2b[:, 0, :], hidT[:, :], True, True, [r_cst, r_hid], [C.r_pb[5]])
                P.cp('act', kcT[:, g, :], C.pb[5][:, 0:NCC * 128], [C.r_pb[5]], [r_kc])
            else:
                for c in range(NCC):
                    P.mm(C.pb[5][:, c * 128:(c + 1) * 128], hidT[:, c * 128:(c + 1) * 128], w2b[:, 1, :], True, True,
                         [r_cst, r_hid], [C.r_pb[5]])
                P.cp('act', vc[:, :, g, :], C.pb[5][:, 0:NCC * 128].rearrange("p (c d) -> p c d", c=NCC), [C.r_pb[5]], [r_kc])

    nsl = 0
    ntc = 0
    net = 0
    ntab = 0

    def dense_branch(h0, a_list, kTile, vTile, r_k, r_v, use_sel, tabidx, bias_col, gate_off, tab, r_tabx):
        nonlocal net
        last = len(a_list) - 1
        base = net
        net += len(a_list)

        def emit_scores(idx, a):
            sbk = (base + idx) % 2
            ti = tabidx(a)
            P.mm(C.pb[sbk][:, 0:512].rearrange("p (h t) -> p h t", h=4), kTile(idx, a), qT[:, h0:h0 + 4, :], True,
                 (not use_sel) and ti is None, [r_k, r_qT], [C.r_pb[sbk]])
            if use_sel:
                P.mm(C.pb[sbk][:, 0:512].rearrange("p (h t) -> p h t", h=4), identb[:, :],
                     selexp[:, a, :].unsqueeze(1).to_broadcast([128, 4, 128]), False, ti is None,
                     [r_identb, r_selexp], [C.r_pb[sbk]])
            if ti is not None:
                P.mm(C.pb[sbk][:, 0:512].rearrange("p (h t) -> p h t", h=4), identb[:, :], tab[:, ti, :, :], False, True,
                     [r_identb, r_tabx], [C.r_pb[sbk]])
            if bias_col is not None:
                P.act(ET[sbk][:, :], C.pb[sbk][:, 0:512], AF.Exp, [C.r_pb[sbk], r_cst], [r_ET[sbk]], bias=bias_col(idx))
            else:
                P.act(ET[sbk][:, :], C.pb[sbk][:, 0:512], AF.Exp, [C.r_pb[sbk]], [r_ET[sbk]])

        def emit_pv(idx, a):
            sbk = (base + idx) % 2
            for hq in range(4):
                bk = 2 + hq
                P.mm(C.pb[bk][:, 0:129], ET[sbk][:, hq * 128:(hq + 1) * 128], vTile(idx, a), idx == 0, idx == last,
                     [r_ET[sbk], r_v], [C.r_pb[bk]])

        for idx, a in enumerate(a_list):
            emit_scores(idx, a)
            if idx > 0:
                emit_pv(idx - 1, a_list[idx - 1])
        emit_pv(last, a_list[last])
        for hq in range(4):
            h = h0 + hq
            bk = 2 + hq
            off = 0
            P.op('dve', lambda e, bk=bk, off=off: e.reciprocal(out=sm[:, 0:1], in_=C.pb[bk][:, off + 128:off + 129]),
                 [C.r_pb[bk]], [r_sm])
            P.tt('dve', sm[:, 0:1], sm[:, 0:1], gates[:, 3 * h + gate_off:3 * h + gate_off + 1], ALU.mult, [r_sm, r_gates], [r_sm])
            P.stt('dve', ocat[:, h * 128:(h + 1) * 128], C.pb[bk][:, off:off + 128], sm[:, 0:1], ocat[:, h * 128:(h + 1) * 128],
                  ALU.mult, ALU.add, [C.r_pb[bk], r_sm, r_ocat], [r_ocat])

    for j in range(NQB):
        amax = NBA - NQB + j
        NA = amax + 1
        coff = 8 * (NQB - 1 - j)
        P.dma('sp', qT[:, :, :], q_scr[:, :, j * 128:(j + 1) * 128], writes=[r_qT])
        P.dma('sp', gates[:, :], g_scr[j * 128:(j + 1) * 128, :], writes=[r_gates])
        P.dma('sp', maskC[:, :], maskC_d[j], writes=[r_slot])
        P.dma('sp', selt[:, :, :], selt_d[j], writes=[r_slot])
        for g in range(G):
            P.dma('sp', ksT[:, 0:NA * 128], kT_d[:, 2, g, 0:NA * 128], writes=[r_ks])
            P.dma('sp', vs[:, 0:NA, 0:128], vt_d[0:NA * 128, 0, g * 128:(g + 1) * 128].rearrange("(a p) d -> p a d", p=128),
                  writes=[r_vs])
            P.dma('sp', kwT[:, :], kT_d[:, 3, g, (amax - 4) * 128:(amax + 1) * 128], writes=[r_kw])
            P.dma('sp', vw[:, :, 0:128],
                  vt_d[(amax - 4) * 128:(amax + 1) * 128, 1, g * 128:(g + 1) * 128].rearrange("(a p) d -> p a d", p=128),
                  writes=[r_vw])
            for hl in range(HG):
                h = g * HG + hl
                tcs = ntc % 2
                ntc += 1
                P.dma('sp', tabC[tcs][:, :], tabC_d[:, h, :], writes=[r_tabC[tcs]])
                P.mm(C.pb[4][:, 0:NCMP], qT[:, h, :], kcT[:, g, 0:NCMP], True, True, [r_qT, r_kc], [C.r_pb[4]])
                P.tt('dve', Ssb[:, :], C.pb[4][:, 0:NCMP], tabC[tcs][:, coff:coff + NCMP], ALU.add, [C.r_pb[4], r_tabC[tcs]], [r_Ssb])
                P.tt('dve', Ssb[:, :], Ssb[:, :], maskC[:, :], ALU.add, [r_Ssb, r_slot], [r_Ssb])
                P.op('dve', lambda e: e.tensor_reduce(out=sm[:, 1:2], in_=Ssb[:, :], axis=AX.X, op=ALU.max), [r_Ssb], [r_sm])
                P.ts('dve', sm[:, 1:2], sm[:, 1:2], -1e4, -1.0, ALU.max, ALU.mult, [r_sm], [r_sm])
                P.act(e32[:, 0:NCMP], Ssb[:, :], AF.Exp, [r_Ssb, r_sm], [r_e32, r_sm], bias=sm[:, 1:2], accum_out=sm[:, 2:3])
                P.ts('dve', sm[:, 2:3], sm[:, 2:3], 1e-30, None, ALU.add, None, [r_sm], [r_sm])
                P.op('dve', lambda e: e.reciprocal(out=sm[:, 2:3], in_=sm[:, 2:3]), [r_sm], [r_sm])
                if hl == 0:
                    P.ts('dve', imp[:, :], e32[:, 0:NCMP], sm[:, 2:3], None, ALU.mult, None, [r_e32, r_sm], [r_imp])
                else:
                    P.stt('dve', imp[:, :], e32[:, 0:NCMP], sm[:, 2:3], imp[:, :], ALU.mult, ALU.add, [r_e32, r_sm, r_imp], [r_imp])
                for c in range(NCC):
                    P.tr(C.pb[5][:, c * 128:(c + 1) * 128], e32[:, c * 128:(c + 1) * 128], C.ident[:, :], [r_e32, C.r_ident], [C.r_pb[5]])
                P.cp('act', eT[:, :, :], C.pb[5][:, 0:NCC * 128].rearrange("p (c t) -> p c t", c=NCC), [C.r_pb[5]], [r_eT])
                for c in range(NCC):
                    P.mm(C.pb[6][:, 0:128], eT[:, c, :], vc[:, c, g, :], c == 0, c == NCC - 1, [r_eT, r_kc], [C.r_pb[6]])
                P.tt('dve', sm[:, 3:4], sm[:, 2:3], gates[:, 3 * h:3 * h + 1], ALU.mult, [r_sm, r_gates], [r_sm])
                P.ts('dve', ocat[:, h * 128:(h + 1) * 128], C.pb[6][:, 0:128], sm[:, 3:4], None, ALU.mult, None,
                     [C.r_pb[6], r_sm], [r_ocat])
            isl, sco, wrk, sel = scr[:, 0, :], scr[:, 1, :], scr[:, 2, :], scr[:, 3, :]
            P.op('dve', lambda e: e.tensor_reduce(out=isl, in_=imp[:, :].rearrange("p (j m) -> p j m", m=4), axis=AX.X, op=ALU.add),
                 [r_imp], [r_scr])
            P.tt('dve', scr[:, 0, 1:NSB], scr[:, 0, 1:NSB], imp[:, 3:NCMP - 4:4], ALU.add, [r_scr, r_imp], [r_scr])
            P.tt('dve', sco, isl, selt[:, 0, :], ALU.mult, [r_scr, r_slot], [r_scr])
            P.tt('dve', sco, sco, selt[:, 1, :], ALU.add, [r_scr, r_slot], [r_scr])
            P.op('dve', lambda e: e.max(out=top8[:, 0:8], in_=sco), [r_scr], [r_sm])
            nsel = min(16, NSB)
            if nsel > 8:
                P.op('dve', lambda e: e.match_replace(out=wrk, in_to_replace=top8[:, 0:8], in_values=sco, imm_value=-3e38),
                     [r_scr, r_sm], [r_scr])
                P.op('dve', lambda e: e.max(out=top8[:, 8:16], in_=wrk), [r_scr], [r_sm])
                P.op('dve', lambda e: e.tensor_reduce(out=sm[:, 4:5], in_=top8[:, 8:16], axis=AX.X, op=ALU.min), [r_sm], [r_sm])
            else:
                P.op('dve', lambda e: e.tensor_reduce(out=sm[:, 4:5], in_=top8[:, 0:8], axis=AX.X, op=ALU.min), [r_sm], [r_sm])
            P.ts('dve', sel, sco, sm[:, 4:5], None, ALU.is_ge, None, [r_scr, r_sm], [r_scr])
            P.tt('dve', sel, sel, selt[:, 2, :], ALU.mult, [r_scr, r_slot], [r_scr])
            P.ts('dve', sel, sel, -MNEG, MNEG, ALU.mult, ALU.add, [r_scr], [r_scr])
            P.tr(C.pb[5][0:NSB, 0:128], sel, C.ident[:, :], [r_scr, C.r_ident], [C.r_pb[5]])
            P.cp('act', MaddT[:, :], C.pb[5][0:NSB, 0:128], [C.r_pb[5]], [r_MaddT])
            for a0 in range(0, NA, 4):
                an = min(4, NA - a0)
                for q in range(an):
                    P.mm(C.pb[6][:, q * 128:(q + 1) * 128], Ex[:, a0 + q, :], MaddT[:, :], True, True, [r_cst, r_MaddT], [C.r_pb[6]])
                P.cp('act', selexp[:, a0:a0 + an, :], C.pb[6][:, 0:an * 128].rearrange("p (a t) -> p a t", a=an),
                     [C.r_pb[6]], [r_selexp])
            for hh in range(HG // 4):
                h0 = g * HG + hh * 4
                qi = h0 // 4
                if qi == 0:
                    P.dma('sp', tabs[ntab % 2][:, :, :, :], tabF_d[0], reads=[r_tabF[0]], writes=[r_tabs[ntab % 2]])
                tab, r_tabq = tabs[ntab % 2], r_tabs[ntab % 2]
                ntab += 1
                if qi + 1 < H // 4:
                    P.dma('sp', tabs[ntab % 2][:, :, :, :], tabF_d[qi + 1], reads=[r_tabF[qi + 1]], writes=[r_tabs[ntab % 2]])
                dense_branch(h0, list(range(NA)), lambda idx, a: ksT[:, a * 128:(a + 1) * 128], lambda idx, a: vs[:, a, :],
                             r_ks, r_vs, True, (lambda a, amax=amax: (amax - a) if amax - a <= 7 else None), None, 1, tab, r_tabq)
                dense_branch(h0, list(range(amax - 4, amax + 1)), lambda idx, a: kwT[:, idx * 128:(idx + 1) * 128],
                             lambda idx, a: vw[:, idx, :], r_kw, r_vw, False,
                             (lambda a, amax=amax: (amax - a) if amax - a <= 3 else 8),
                             (lambda idx, j=j: wpad[:, j, idx:idx + 1]), 2, tab, r_tabq)
        P.dma('sp', o_scr[j * 128:(j + 1) * 128, :], ocat[:, :], reads=[r_ocat])
    P.end_phase(m2)

    oT = P.sb("oT", [128, H, TTP], BF16)
    r_oT = P.res()
    osrc = P.sb("osrc", [128, QW], F32)
    r_osrc = P.res()
    y = [P.sb(f"y{i}", [128, D], F32) for i in range(NBP)]
    r_y = [P.res() for _ in range(NBP)]
    junk3 = P.sb("junk3", [128, D], BF16)
    r_junk3 = P.res()
    wob = [P.sb(f"wob{i}", [128, KS, DW], BF16) for i in range(2)]
    r_wob = [P.res(), P.res()]
    nsl = 0
    for ti in range(NT // TTP):
        for b in range(NBP):
            blk = ti * NBP + b
            P.dma('sp', osrc[:, :], o_scr[blk * 128:(blk + 1) * 128, :], writes=[r_osrc])
            for k0 in range(0, H, 4):
                bk = (k0 // 4) % 2
                for q in range(4):
                    P.tr(C.pb[bk][:, q * 128:(q + 1) * 128], osrc[:, (k0 + q) * 128:(k0 + q + 1) * 128], C.ident[:, :],
                         [r_osrc, C.r_ident], [C.r_pb[bk]])
                P.cp('act', oT[:, k0:k0 + 4, b * 128:(b + 1) * 128], C.pb[bk][:, 0:512].rearrange("p (k t) -> p k t", k=4),
                     [C.r_pb[bk]], [r_oT])
        for n in range(ND):
            for k0 in range(0, H, KS):
                kn = min(KS, H - k0)
                ws = nsl % 2
                nsl += 1
                P.dma('pool', wob[ws][:, 0:kn, :], woutv[:, k0:k0 + kn, n * DW:(n + 1) * DW], writes=[r_wob[ws]])
                for b in range(NBP):
                    for k in range(kn):
                        kc = k0 + k
                        P.mm(C.pb[4 + b][:, 0:DW], oT[:, kc, b * 128:(b + 1) * 128], wob[ws][:, k, :], kc == 0, kc == H - 1,
                             [r_oT, r_wob[ws]], [C.r_pb[4 + b]])
            for b in range(NBP):
                P.cp('act', y[b][:, n * DW:(n + 1) * DW], C.pb[4 + b][:, 0:DW], [C.r_pb[4 + b]], [r_y[b]])
        for b in range(NBP):
            blk = ti * NBP + b
            postnorm_block(C, y[b], r_y[b], x_d[blk * 128:(blk + 1) * 128, :], xt, r_xt, junk3, r_junk3, stat, r_stat,
                           GP, r_GP, o_d[blk * 128:(blk + 1) * 128, :])
    if own:
        P.emit()
        return nc
    P.end_phase(m_phase)
    return None


def build_fused(D, S, F, H, G, ncores):
    nc = bass.Bass("TRN2", target_bir_lowering=False)
    P = Prog(nc)
    C = Common(P, nc, D)
    ctx = (nc, P, C)
    NT = S // ncores
    NQB = NT // 128
    KC = D // 128
    w = D // ncores
    NCOL = 14 * w
    GW = G * 128
    WQ = w // 128
    idt = lambda name, shape, ty: nc.dram_tensor(name, list(shape), ty, kind="Internal").ap()
    modT = P.sb("modT", [128, 14, KC], F32)
    r_modT = P.res()
    hsel = P.sb("hsel", [2 * ncores, 2], F32)
    shw = P.sb("shw", [128, ncores], F32)
    r_g = P.res()
    P.dma('sp', hsel[:], nc.dram_tensor("hsel", [2 * ncores, 2], F32, kind="ExternalInput").ap(), writes=[r_g])
    P.dma('sp', shw[:], nc.dram_tensor("shw", [128, ncores], F32, kind="ExternalInput").ap(), writes=[r_g])

    mod_loc = idt("mod_loc", [1, NCOL], F32)
    modg = idt("modg", [ncores, NCOL], F32)
    r_modg = Res()
    build_gemv(D, NCOL, ctx=ctx, io={"o": mod_loc}, pfx="g_")
    P.pfx = "m0_"
    P.collective("AllGather", mod_loc, modg, ncores, writes=[r_modg])
    m = P.mark()
    mg = P.sb("mg", [ncores, NCOL], F32)
    r_mg = P.res()
    P.dma('sp', mg[:], modg, reads=[r_modg], writes=[r_mg])
    for pidx in range(14):
        for q in range(WQ):
            P.tr(C.pb[pidx % 2][:, q * ncores:(q + 1) * ncores], mg[0:ncores, pidx * w + q * 128:pidx * w + (q + 1) * 128],
                 C.ident[0:ncores, 0:ncores], [r_mg, C.r_ident], [C.r_pb[pidx % 2]])
        P.cp('dve', modT[:, pidx, :].rearrange("p (r q) -> p q r", q=WQ),
             C.pb[pidx % 2][:, 0:WQ * ncores].rearrange("p (q r) -> p q r", q=WQ), [C.r_pb[pidx % 2]], [r_modT])
    P.end_phase(m)
    fz = lambda idx: (modT, r_modT, modg, r_modg, idx, ncores)

    def emit_halo(x_scr, tag):
        P.pfx = tag
        m = P.mark()
        hb_in = idt(tag + "hb_in", [2, D], F32)
        hb_out = idt(tag + "hb_out", [2 * ncores, D], F32)
        xh0 = idt(tag + "xh0", [2, D], F32)
        t2 = P.sb("t2", [2, D], F32)
        r_t2 = P.res()
        rows = P.sb("rows", [2 * ncores, D], F32)
        r_rows = P.res()
        r_hb = Res()
        P.dma('sp', t2[:], x_scr[NT - 2:NT, :], writes=[r_t2])
        P.dma('sp', hb_in, t2[:], reads=[r_t2], writes=[r_hb])
        P.collective("AllGather", hb_in, hb_out, ncores, reads=[r_hb], writes=[r_hb])
        P.dma('sp', rows[:], hb_out, reads=[r_hb], writes=[r_rows])
        for c in range(0, D, 512):
            cw_ = min(512, D - c)
            P.mm(C.pb[0][0:2, 0:cw_], hsel[:, :], rows[:, c:c + cw_], True, True, [r_g, r_rows], [C.r_pb[0]])
            P.cp('dve', t2[:, c:c + cw_], C.pb[0][0:2, 0:cw_], [C.r_pb[0]], [r_t2])
        P.dma('sp', xh0, t2[:], reads=[r_t2])
        P.end_phase(m)
        return xh0

    x1 = idt("x1_scr", [NT, D], F32)
    x2 = idt("x2_scr", [NT, D], F32)
    x3 = idt("x3_scr", [NT, D], F32)
    build_gmlp(D, NT, ctx=ctx, io={"out": x1}, pfx="a_", fz=fz((0, 1, 2)))
    xh1 = emit_halo(x1, "h1_")
    build_ffn2(D, F, NT, ctx=ctx, io={"x": x1, "out": x2}, pfx="f0_", fz=fz((3, 4, 5)),
               halo_src=lambda blk: xh1 if blk == 0 else x1[blk * 128 - 2:blk * 128, :])
    CT = 4 * G * NT
    kT_loc = idt("kT_loc", [128, 4, G, NT], BF16)
    vt_loc = idt("vt_loc", [NT, 2, GW], BF16)
    kTg = idt("kTg", [ncores * 128, CT], BF16)
    vtg = idt("vtg", [ncores * NT, 2 * GW], BF16)
    kTall = idt("kTall_s", [128, 4, G, S], BF16)
    vtall = idt("vtall_s", [S, 2, GW], BF16)
    build_kv(D, G, NT, ctx=ctx, io={"x": x2, "kT": kT_loc, "vtok": vt_loc}, pfx="kv_", fz=fz((12, 13, None)))
    P.pfx = "sh_"
    r_kvg = Res()
    P.collective("AllGather", kT_loc.rearrange("p a g t -> p (a g t)"), kTg, ncores, writes=[r_kvg])
    P.collective("AllGather", vt_loc.rearrange("t a c -> t (a c)"), vtg, ncores, writes=[r_kvg])
    m = P.mark()
    Isel = P.sb("Isel", [128, ncores, 128], BF16)
    r_Isel = P.res()
    for k in range(ncores):
        P.ts('dve', Isel[:, k, :], C.ident[:, :], shw[:, k:k + 1], None, ALU.mult, None, [C.r_ident, r_g], [r_Isel])
    kb = [[P.sb(f"kb{s_}_{i}", [128, NT], BF16) for i in range(ncores)] for s_ in range(2)]
    r_kb = [[P.res() for i in range(ncores)] for s_ in range(2)]
    ost = [P.sb(f"ost{i}", [128, NT], BF16) for i in range(2)]
    r_ost = [P.res(), P.res()]
    n_it = 0
    a_win0 = S // 128 - NQB - 4
    for p in range(ncores):
        for kg in range(4 * G):
            if kg // G == 3 and (p + 1) * NQB - 1 < a_win0:
                continue
            st_ = n_it % 2
            n_it += 1
            cands = [k for k in range(ncores) if p - k >= 0]
            for idx, k in enumerate(cands):
                P.dma('sp' if idx % 2 == 0 else 'pool', kb[st_][idx][:, :], kTg[(p - k) * 128:(p - k + 1) * 128, kg * NT:(kg + 1) * NT],
                      reads=[r_kvg], writes=[r_kb[st_][idx]])
            for sub in range(NT // 512):
                bk = 2 + (sub % 2)
                for idx, k in enumerate(cands):
                    P.mm(C.pb[bk][:, 0:512], Isel[:, k, :], kb[st_][idx][:, sub * 512:(sub + 1) * 512], idx == 0, idx == len(cands) - 1,
                         [r_Isel, r_kb[st_][idx]], [C.r_pb[bk]])
                P.cp('act' if sub % 2 == 0 else 'dve', ost[st_][:, sub * 512:(sub + 1) * 512], C.pb[bk][:, 0:512], [C.r_pb[bk]], [r_ost[st_]])
            P.dma('sp', kTall[:, kg // G, kg % G, p * NT:(p + 1) * NT], ost[st_][:, :], reads=[r_ost[st_]])
    for a in range(S // 128):
        st_ = n_it % 2
        n_it += 1
        VW = 2 * GW if a >= a_win0 else GW
        cands = [k for k in range(ncores) if a - NQB * k >= 0]
        for idx, k in enumerate(cands):
            P.dma('sp' if idx % 2 == 0 else 'pool', kb[st_][idx][:, 0:VW], vtg[(a - NQB * k) * 128:(a - NQB * k + 1) * 128, 0:VW],
                  reads=[r_kvg], writes=[r_kb[st_][idx]])
        TW = min(512, VW)
        for sub in range(VW // TW):
            bk = 2 + (sub % 2)
            for idx, k in enumerate(cands):
                P.mm(C.pb[bk][:, 0:TW], Isel[:, k, :], kb[st_][idx][:, sub * TW:(sub + 1) * TW], idx == 0, idx == len(cands) - 1,
                     [r_Isel, r_kb[st_][idx]], [C.r_pb[bk]])
            P.cp('act' if sub % 2 == 0 else 'dve', ost[st_][:, sub * TW:(sub + 1) * TW], C.pb[bk][:, 0:TW], [C.r_pb[bk]], [r_ost[st_]])
        P.dma('sp', vtall[a * 128:(a + 1) * 128, :, :].rearrange("t a c -> t (a c)")[:, 0:VW], ost[st_][:, 0:VW], reads=[r_ost[st_]])
    P.end_phase(m)
    build_attn(D, S, H, G, NQB, ctx=ctx, io={"x": x2, "out": x3, "kTall": kTall, "vtall": vtall}, pfx="b_", fz=fz((6, 7, 8)))
    xh3 = emit_halo(x3, "h3_")
    build_ffn2(D, F, NT, ctx=ctx, io={"x": x3}, pfx="f1_", fz=fz((9, 10, 11)),
               halo_src=lambda blk: xh3 if blk == 0 else x3[blk * 128 - 2:blk * 128, :])
    P.pfx = "fin_"
    P.emit()
    return nc


import math


def t5_bucket_np(rel):
    n = np.maximum(rel, 0)
    nf = np.maximum(n, 16).astype(np.float32)
    large = 16 + (np.log(nf / np.float32(16)) / np.float32(math.log(64.0)) * np.float32(16)).astype(np.int32)
    large = np.minimum(large, 31)
    return np.where(n < 16, n, large).astype(np.int64)


def attn_static(S, NQB, ncores):
    NBA, NCMP, NSB = S // 128, S // 16, S // 64
    NTC = NCMP + 8 * (NQB - 1)
    s_ = np.arange(128)[:, None, None]
    t_ = np.arange(128)[None, None, :]
    k_ = np.arange(9)[None, :, None]
    relT = np.where(k_ < 8, 128 * k_, 512) + t_ - s_
    idxT = t5_bucket_np(relT)
    M0 = 8 * (NBA - 1)
    relC = 16 * (M0 - np.arange(NTC)[None, :]) + np.arange(128)[:, None] - 31
    idxC = t5_bucket_np(relC)
    sI, tI = np.arange(128)[:, None], np.arange(128)[None, :]
    maskD0 = np.where(tI >= sI, 0.0, MNEG).astype(np.float32)
    maskW4 = np.where(tI < sI, 0.0, MNEG).astype(np.float32)
    Ex = np.zeros((NSB, NBA, 128), np.float32)
    for a in range(NBA):
        Ex[2 * a, a, 0:64] = 1.0
        Ex[2 * a + 1, a, 64:128] = 1.0
    per_core = []
    for i in range(ncores):
        PADB = (ncores - 1 - i) * NQB
        maskC = np.zeros((NQB, 128, NCMP), np.float32)
        selt = np.zeros((NQB, 128, 3, NSB), np.float32)
        wpad = np.zeros((128, NQB, 5), np.float32)
        for j in range(NQB):
            amax = NBA - NQB + j
            tl = np.arange(128)[:, None]
            na = np.arange(NCMP)[None, :]
            rel = 16 * (8 * amax - na) + tl - 31
            maskC[j] = np.where((rel >= 0) & (na >= 8 * PADB), 0.0, -1e30)
            ja = np.arange(NSB)[None, :]
            cur = 2 * amax + (tl >= 64)
            lo = 2 * PADB
            valid = (ja >= lo) & (ja <= cur)
            Aadd = np.where(valid, 0.0, -1e30)
            Aadd = np.where(valid & (ja == cur - 1), 0.8e30, Aadd)
            Aadd = np.where(valid & (ja == cur), 0.9e30, Aadd)
            Aadd = np.where(valid & (ja == lo), 1e30, Aadd)
            selt[j, :, 0, :] = valid
            selt[j, :, 1, :] = Aadd
            selt[j, :, 2, :] = valid
            for idx in range(5):
                wpad[:, j, idx] = 0.0 if (amax - 4 + idx) >= PADB else MNEG
        per_core.append(dict(maskC=maskC, seltab=selt, wpad=wpad))
    return dict(idxT=idxT, idxC=idxC, maskD0=maskD0, maskW4=maskW4, Ex=Ex, per_core=per_core)


def attn_bias_tables(rel_bias, st):
    rb = np.asarray(rel_bias, np.float32)
    tabT = np.ascontiguousarray(rb[st['idxT']].transpose(0, 1, 3, 2))
    tabC = np.ascontiguousarray(rb[st['idxC']].transpose(0, 2, 1))
    c31 = np.ascontiguousarray(np.broadcast_to(rb[31][None, :], (128, rb.shape[1])))
    return tabT, tabC, c31


def shift_right(arr, axis, pad_elems):
    out = np.zeros_like(arr)
    n = arr.shape[axis]
    if pad_elems >= n:
        return out
    src = [slice(None)] * arr.ndim
    dst = [slice(None)] * arr.ndim
    src[axis] = slice(0, n - pad_elems)
    dst[axis] = slice(pad_elems, n)
    out[tuple(dst)] = arr[tuple(src)]
    return out


NCORES = 8
D_MODEL, SEQ, D_FF, N_HEADS, N_GROUPS = 4096, 8192, 11008, 32, 4


def slab_major(wm, cw):
    K, N = wm.shape
    return np.ascontiguousarray(wm.reshape(K // 128, 128, N // cw, cw).transpose(2, 1, 0, 3))


def fused_in_maps(inp, D, S, F, H, G, ncores):
    f32 = lambda a: np.ascontiguousarray(np.asarray(a, dtype=np.float32))
    NT = S // ncores
    NQB = NT // 128
    w = D // ncores
    QW = H * 128
    x0 = f32(inp["x"])[0]
    mod_w, mod_b = f32(inp["mod_w"]), f32(inp["mod_b"])
    kv_mod_w, kv_mod_b = f32(inp["kv_mod_w"]), f32(inp["kv_mod_b"])
    parts = []
    for l in range(2):
        for sidx in range(2):
            for j in range(3):
                parts.append((mod_w[l, sidx][:, j * D:(j + 1) * D], mod_b[l, sidx][j * D:(j + 1) * D]))
    for j in range(2):
        parts.append((kv_mod_w[:, j * D:(j + 1) * D], kv_mod_b[j * D:(j + 1) * D]))
    norm_pre, norm_post = f32(inp["norm_pre"]), f32(inp["norm_post"])
    a_b_in = f32(inp["a_b_in"])[0]
    b_b_in = f32(inp["b_b_in"])[0]
    cmp_pe, cmp_b1 = f32(inp["cmp_pe"]), f32(inp["cmp_b1"])
    f_conv_w, f_conv_b = f32(inp["f_conv_w"]), f32(inp["f_conv_b"])
    st = attn_static(S, NQB, ncores)
    tabT, tabC, c31 = attn_bias_tables(f32(inp["rel_bias"]), st)
    shared = {
        "ident": np.eye(128, dtype=np.float32), "g_cT": cols(f32(inp["c"])[0]),
        "a_a_w_in_u": slab_major(f32(inp["a_w_in"])[0][:, :D], 256 if D % 256 == 0 else 128),
        "a_a_w_in_v": np.ascontiguousarray(f32(inp["a_w_in"])[0][:, D:]), "a_a_b_in_c": cols(a_b_in[:D]), "a_a_b_in_r": np.ascontiguousarray(a_b_in[None, D:]),
        "a_a_ln_c": np.stack([cols(f32(inp["a_ln_g"])[0]), cols(f32(inp["a_ln_b"])[0])], 1), "a_a_w_s": f32(inp["a_w_s"])[0],
        "a_a_b_s": f32(inp["a_b_s"])[0].reshape(1, -1), "a_a_w_out": f32(inp["a_w_out"])[0],
        "a_triu": np.triu(np.ones((128, 128), np.float32)),
        "a_m_gpre": cols(norm_pre[0, 0]), "a_m_gpost": np.ascontiguousarray(norm_post[0, 0][None, :]),
        "kv_w_kv": f32(inp["w_kv"]), "kv_w_kv_k": slab_major(f32(inp["w_kv"]), 128), "kv_kvnorm": cols(f32(inp["kv_norm"])),
        "b_b_w_in": f32(inp["b_w_in"])[0], "b_b_w_in_q": slab_major(f32(inp["b_w_in"])[0][:, :QW], 128), "b_b_bq": cols(b_b_in[:QW]), "b_b_bg": np.ascontiguousarray(b_b_in[None, QW:]),
        "b_b_w_out": f32(inp["b_w_out"])[0], "b_cmp_peT": np.ascontiguousarray(cmp_pe.transpose(2, 0, 1)),
        "b_cmp_w1": f32(inp["cmp_w1"]), "b_cmp_b1c": np.ascontiguousarray(cmp_b1.T), "b_cmp_w2": f32(inp["cmp_w2"]),
        "b_tabT": tabT, "b_tabC": tabC, "b_c31": c31, "b_maskD0": st["maskD0"], "b_maskW4": st["maskW4"], "b_Ex": st["Ex"],
        "b_b_gpre": cols(norm_pre[1, 0]), "b_b_gpost": np.ascontiguousarray(norm_post[1, 0][None, :]),
    }
    for l in range(2):
        p = f"f{l}_"
        shared[p + "w_gate"] = slab_major(f32(inp["f_w_gate"])[l], 128)
        shared[p + "w_up"] = slab_major(f32(inp["f_w_up"])[l], 128)
        shared[p + "w_down"] = f32(inp["f_w_down"])[l]
        shared[p + "convc"] = np.stack([cols(f_conv_w[l, 0]), cols(f_conv_w[l, 1]), cols(f_conv_w[l, 2]), cols(f_conv_b[l])], 1)
        shared[p + "f_gpre"] = cols(norm_pre[l, 1])
        shared[p + "f_gpost"] = np.ascontiguousarray(norm_post[l, 1][None, :])
    in_maps = []
    for i in range(ncores):
        im = dict(shared)
        im["a_x"] = x0[i * NT:(i + 1) * NT]
        im["g_W"] = np.ascontiguousarray(np.concatenate([Wp[:, i * w:(i + 1) * w] for Wp, _ in parts], 1))
        im["g_b"] = np.concatenate([bp[i * w:(i + 1) * w] for _, bp in parts])[None, :]
        hsel = np.zeros((2 * ncores, 2), np.float32)
        if i >= 1:
            hsel[2 * (i - 1), 0] = 1.0
            hsel[2 * (i - 1) + 1, 1] = 1.0
        im["hsel"] = hsel
        shw = np.zeros((128, ncores), np.float32)
        shw[:, ncores - 1 - i] = 1.0
        im["shw"] = shw
        hm = np.ones((128, NQB * 2), np.float32)
        if i == 0:
            hm[:, 0:2] = 0.0
        im["f0_hmask"] = hm
        im["f1_hmask"] = hm
        pc = st['per_core'][i]
        im["b_maskC"], im["b_seltab"], im["b_wpad"] = pc["maskC"], pc["seltab"], pc["wpad"]
        in_maps.append(im)
    return in_maps


_NC_CACHE = {}


def kernel(**inputs):
    D, S, F, H, G, n = D_MODEL, SEQ, D_FF, N_HEADS, N_GROUPS, NCORES
    in_maps = fused_in_maps(inputs, D, S, F, H, G, n)
    if "nc" not in _NC_CACHE:
        _NC_CACHE["nc"] = build_fused(D, S, F, H, G, n)
    res = run_bass_kernel_spmd(_NC_CACHE["nc"], in_maps, core_ids=list(range(n)))
    et = getattr(res, "exec_time_ns", None)
    if et is not None:
        print(f"[fused launch] device exec_time_ns={et}", flush=True)
    out = np.concatenate([r["f1_out"] for r in res.results], 0)
    return out[None].astype(np.float32)
```
